# Optimizing a Trainium2 kernel written in Bass

```python
import math
import jax
import jax.numpy as jnp
from jax import lax
import numpy as np

D_MODEL = 1024
BATCH = 32
SEQ = 2048
DEPTH = 4

CTX_LEN = 256
GRID_W = 64
EPS = 1e-6
ROPE_BASE = 10000.0
Q_BLOCK = 128
F32 = jnp.float32
F_MIN = 1e-30
LB_MAX = 1.0 - 1e-6

A_HEADS = 4
A_DK = 128
A_DV = 128
A_KEY = A_HEADS * A_DK
A_VAL = A_HEADS * A_DV
HGRN_CHUNK = 32

B_HEADS = 4
B_HD = 64
B_QK = B_HEADS * 2 * B_HD
B_VAL = B_HEADS * 2 * B_HD

C_HEADS = 8
C_NOPE = 64
C_ROPE = 32
C_V = 64
C_Q_LORA = 384
C_KV_LORA = 256
C_VAL = C_HEADS * C_V

N_BRANCH = 3
BRANCH_W = 512

N_EXPERTS = 16
EXPERT_FF = 2048
CAPACITY_FACTOR = 2

IN_SPLITS = (A_KEY, A_VAL, A_VAL, A_KEY, A_KEY,
             B_QK, B_QK, B_VAL,
             C_Q_LORA, C_KV_LORA, C_ROPE,
             N_BRANCH * D_MODEL)
N_IN = sum(IN_SPLITS)

kernel_name = 'hybrid_dit_hgrn2_diffattn_mla_ecmoe'


def rmsnorm(x, g):
    xf = x.astype(F32)
    y = xf * lax.rsqrt(jnp.mean(xf * xf, axis=-1, keepdims=True) + EPS)
    return y.astype(x.dtype) * g


def _modulation(vec, w, b):
    m = jax.nn.silu(vec) @ w + b
    return jnp.split(m[..., None, :], 6, axis=-1)


def _modulate(h, shift, scale):
    return h * (1.0 + scale) + shift


def _split_cols(a):
    offs, acc = [], 0
    for w in IN_SPLITS[:-1]:
        acc += w
        offs.append(acc)
    return jnp.split(a, offs, axis=-1)


def rope_2d(rows, rot_dim):
    row = jnp.repeat(jnp.arange(rows, dtype=F32), GRID_W)
    col = jnp.tile(jnp.arange(GRID_W, dtype=F32), rows)
    n_freq = rot_dim // 4
    inv_freq = ROPE_BASE ** (-jnp.arange(n_freq, dtype=F32) / n_freq)
    ang = jnp.concatenate([row[:, None] * inv_freq, col[:, None] * inv_freq], axis=-1)
    return jnp.cos(ang), jnp.sin(ang)


def apply_rope(x, cos, sin):
    shape = (cos.shape[0],) + (1,) * (x.ndim - 3) + (cos.shape[1],)
    c = cos.reshape(shape).astype(x.dtype)
    s = sin.reshape(shape).astype(x.dtype)
    x1, x2 = jnp.split(x, 2, axis=-1)
    return jnp.concatenate([x1 * c - x2 * s, x1 * s + x2 * c], axis=-1)


def _query_blocks(fn, *qs):
    B, T = qs[0].shape[:2]
    nb = T // Q_BLOCK
    blocks = tuple(jnp.moveaxis(q.reshape(B, nb, Q_BLOCK, *q.shape[2:]), 1, 0) for q in qs)
    out = lax.map(lambda bl: fn(*bl), blocks)
    return jnp.moveaxis(out, 0, 1).reshape(B, T, *out.shape[3:])


def softmax_attention(q, k, v, scale):
    def block(qb):
        s = jnp.einsum('bqhd,bkhd->bhqk', qb, k).astype(F32) * scale
        p = jax.nn.softmax(s, axis=-1).astype(v.dtype)
        return jnp.einsum('bhqk,bkhd->bqhd', p, v)
    return _query_blocks(block, q)


def diff_attention(q1, q2, k1, k2, v, lam, scale):
    def block(q1b, q2b):
        p1 = jax.nn.softmax(jnp.einsum('bqhd,bkhd->bhqk', q1b, k1).astype(F32) * scale, axis=-1)
        p2 = jax.nn.softmax(jnp.einsum('bqhd,bkhd->bhqk', q2b, k2).astype(F32) * scale, axis=-1)
        return jnp.einsum('bhqk,bkhd->bqhd', (p1 - lam * p2).astype(v.dtype), v)
    return _query_blocks(block, q1, q2)


def hgrn2_chunk_scan(q, k, v, log_f, s0):
    B, T, H, DK = q.shape
    DV = v.shape[-1]
    n = T // HGRN_CHUNK

    def chunks(a):
        return jnp.moveaxis(a.reshape(B, n, HGRN_CHUNK, H, a.shape[-1]), 1, 0)

    causal = jnp.tril(jnp.ones((HGRN_CHUNK, HGRN_CHUNK), dtype=bool))[None, :, :, None, None]

    def step(state, inp):
        qc, kc, vc, lf = inp
        b = jnp.cumsum(lf, axis=1)
        o_inter = jnp.einsum('blhk,bhkv->blhv', qc * jnp.exp(b), state)
        diff = jnp.where(causal, b[:, :, None] - b[:, None, :], 0.0)
        decay = jnp.where(causal, jnp.exp(diff), 0.0)
        scores = jnp.einsum('btshk,bshk->bhts', qc[:, :, None] * decay, kc)
        o_intra = jnp.einsum('bhts,bshv->bthv', scores, vc)
        b_last = b[:, -1]
        state = (jnp.exp(b_last)[..., None] * state
                 + jnp.einsum('bshk,bshv->bhkv', kc * jnp.exp(b_last[:, None] - b), vc))
        return state, o_inter + o_intra

    s_final, o = lax.scan(step, s0, (chunks(q), chunks(k), chunks(v), chunks(log_f)))
    return jnp.moveaxis(o, 0, 1).reshape(B, T, H, DV), s_final


def _flip(a, rev):
    return a[:, ::-1] if rev else a


def hgrn2_branch(cols_l, cols_c, lower_bound, norm_g, with_ctx):
    dtype = cols_l[0].dtype
    lb = lower_bound.reshape(2, A_HEADS, A_DK)

    def prep(q, i, f, lbd):
        B, T = q.shape[:2]
        z = f.reshape(B, T, A_HEADS, A_DK).astype(F32)
        k = (1.0 - lbd) * jax.nn.sigmoid(-z)
        fg = lbd + (1.0 - lbd) * jax.nn.sigmoid(z)
        log_f = jnp.log(jnp.maximum(fg, F_MIN))
        return (q.reshape(B, T, A_HEADS, A_DK).astype(F32), k,
                i.reshape(B, T, A_HEADS, A_DV).astype(F32), log_f)

    B = cols_c[0].shape[0]
    o_l, o_c = 0.0, 0.0
    for d in range(2):
        rev = d == 1
        ctx_in = tuple(_flip(a, rev) for a in prep(cols_c[0], cols_c[1], cols_c[3 + d], lb[d]))
        lat_in = tuple(_flip(a, rev) for a in prep(cols_l[0], cols_l[1], cols_l[3 + d], lb[d]))
        s0 = jnp.zeros((B, A_HEADS, A_DK, A_DV), F32)
        oc, s_ctx = hgrn2_chunk_scan(*ctx_in, s0)
        ol, _ = hgrn2_chunk_scan(*lat_in, s_ctx)
        o_l = o_l + _flip(ol, rev)
        o_c = o_c + _flip(oc, rev)

    def readout(o, g):
        B_, T = o.shape[:2]
        o = rmsnorm(o, norm_g.reshape(A_HEADS, A_DV).astype(F32))
        return (o.reshape(B_, T, A_VAL) * jax.nn.silu(g.astype(F32))).astype(dtype)

    return readout(o_l, cols_l[2]), (readout(o_c, cols_c[2]) if with_ctx else None)


def diff_branch(cols_l, cols_c, rope, lam_params, norm_g, layer, with_ctx):
    lam_init = 0.8 - 0.6 * math.exp(-0.3 * layer)
    lp = lam_params.astype(F32)
    lam = jnp.exp(jnp.sum(lp[0] * lp[1])) - jnp.exp(jnp.sum(lp[2] * lp[3])) + lam_init
    scale = B_HD ** -0.5

    def heads(q, k, v):
        B, T = q.shape[:2]
        return (q.reshape(B, T, B_HEADS, 2, B_HD), k.reshape(B, T, B_HEADS, 2, B_HD),
                v.reshape(B, T, B_HEADS, 2 * B_HD))

    ql, kl, vl = heads(*cols_l)
    qc, kc, vc = heads(*cols_c)
    cos, sin = rope
    ql = apply_rope(ql, cos, sin)
    kl = apply_rope(kl, cos, sin)
    k_all = jnp.concatenate([kc, kl], axis=1)
    v_all = jnp.concatenate([vc, vl], axis=1)

    def post(o):
        B, T = o.shape[:2]
        o = rmsnorm(o, norm_g.reshape(B_HEADS, 2 * B_HD)) * (1.0 - lam_init)
        return o.reshape(B, T, B_VAL)

    o_l = post(diff_attention(ql[..., 0, :], ql[..., 1, :], k_all[..., 0, :], k_all[..., 1, :],
                              v_all, lam, scale))
    o_c = None
    if with_ctx:
        o_c = post(diff_attention(qc[..., 0, :], qc[..., 1, :], kc[..., 0, :], kc[..., 1, :],
                                  vc, lam, scale))
    return o_l, o_c


def mla_branch(cols_l, cols_c, rope, q_norm_g, w_uq, kv_norm_g, w_ukv, with_ctx):
    cos, sin = rope

    def project(cq, ckv, kr, rotate):
        B, T = cq.shape[:2]
        q = (rmsnorm(cq, q_norm_g) @ w_uq).reshape(B, T, C_HEADS, C_NOPE + C_ROPE)
        kv = (rmsnorm(ckv, kv_norm_g) @ w_ukv).reshape(B, T, C_HEADS, C_NOPE + C_V)
        q_nope, q_rope = q[..., :C_NOPE], q[..., C_NOPE:]
        k_nope, v = kv[..., :C_NOPE], kv[..., C_NOPE:]
        k_rope = kr[:, :, None, :]
        if rotate:
            q_rope = apply_rope(q_rope, cos, sin)
            k_rope = apply_rope(k_rope, cos, sin)
        q = jnp.concatenate([q_nope, q_rope], axis=-1)
        k = jnp.concatenate([k_nope, jnp.broadcast_to(k_rope, (B, T, C_HEADS, C_ROPE))], axis=-1)
        return q, k, v

    ql, kl, vl = project(*cols_l, True)
    qc, kc, vc = project(*cols_c, False)
    scale = (C_NOPE + C_ROPE) ** -0.5
    o_l = softmax_attention(ql, jnp.concatenate([kc, kl], axis=1), jnp.concatenate([vc, vl], axis=1), scale)
    o_l = o_l.reshape(*o_l.shape[:2], C_VAL)
    o_c = None
    if with_ctx:
        o_c = softmax_attention(qc, kc, vc, scale)
        o_c = o_c.reshape(*o_c.shape[:2], C_VAL)
    return o_l, o_c


def merge_branches(branches, gate_cols, w_branch, w_out):
    B, T = gate_cols.shape[:2]
    o = jnp.stack(branches, axis=2)
    y = jnp.einsum('btnc,ncd->btnd', o, w_branch)
    g = jax.nn.sigmoid(gate_cols.astype(F32)).astype(y.dtype).reshape(B, T, N_BRANCH, D_MODEL)
    return jnp.sum(g * y, axis=2) @ w_out


def token_mixer(h_l, h_c, layer, lower_bound, rope_b, rope_c, w_in, hgrn_norm_g, diff_lambda,
                diff_norm_g, mla_q_norm_g, mla_w_uq, mla_kv_norm_g, mla_w_ukv, w_branch, w_out, with_ctx):
    cl = _split_cols(h_l @ w_in)
    cc = _split_cols(h_c @ w_in)
    a_l, a_c = hgrn2_branch(cl[0:5], cc[0:5], lower_bound, hgrn_norm_g, with_ctx)
    b_l, b_c = diff_branch(cl[5:8], cc[5:8], rope_b, diff_lambda, diff_norm_g, layer, with_ctx)
    m_l, m_c = mla_branch(cl[8:11], cc[8:11], rope_c, mla_q_norm_g, mla_w_uq, mla_kv_norm_g, mla_w_ukv, with_ctx)
    out_l = merge_branches((a_l, b_l, m_l), cl[11], w_branch, w_out)
    out_c = merge_branches((a_c, b_c, m_c), cc[11], w_branch, w_out) if with_ctx else None
    return out_l, out_c


def expert_choice_ffn(h, router_w, w_gate, w_up, w_down):
    B, T, D = h.shape
    cap = CAPACITY_FACTOR * T // N_EXPERTS
    affinity = jax.nn.softmax((h @ router_w).astype(F32), axis=-1)
    g, idx = lax.top_k(jnp.swapaxes(affinity, 1, 2), cap)
    xs = jax.vmap(lambda hb, ib: hb[ib])(h, idx)
    a = jnp.einsum('becd,edf->becf', xs, w_gate)
    u = jnp.einsum('becd,edf->becf', xs, w_up)
    y = jnp.einsum('becf,efd->becd', jax.nn.silu(a) * u, w_down) * g[..., None].astype(h.dtype)
    return jax.vmap(lambda ib, yb: jnp.zeros((T, D), yb.dtype).at[ib].add(yb))(idx, y)


def setup_inputs(seed: int = 0) -> dict:
    key = jax.random.key(seed)
    ks = jax.random.split(key, 24)
    L, D = DEPTH, D_MODEL

    def nrm(k, shape, scale):
        return jax.random.normal(k, shape, F32) * scale

    return {
        'x': nrm(ks[0], (BATCH, SEQ, D), 1.0),
        'c': nrm(ks[1], (BATCH, D), 1.0),
        'ctx': nrm(ks[2], (BATCH, CTX_LEN, D), 1.0),
        'c_ctx': nrm(ks[3], (D,), 1.0),
        'ada_w': nrm(ks[4], (L, D, 6 * D), 0.5 * D ** -0.5),
        'ada_b': nrm(ks[5], (L, 6 * D), 0.02),
        'norm_mix_g': 1.0 + nrm(ks[6], (L, D), 0.02),
        'norm_ffn_g': 1.0 + nrm(ks[7], (L, D), 0.02),
        'w_in': nrm(ks[8], (L, D, N_IN), D ** -0.5),
        'hgrn_lb': nrm(ks[9], (L, 2, A_KEY), 0.5),
        'hgrn_norm_g': 1.0 + nrm(ks[10], (L, A_VAL), 0.02),
        'diff_lambda': nrm(ks[11], (L, 4, B_HD), 0.1),
        'diff_norm_g': 1.0 + nrm(ks[12], (L, B_VAL), 0.02),
        'mla_q_norm_g': 1.0 + nrm(ks[13], (L, C_Q_LORA), 0.02),
        'mla_w_uq': nrm(ks[14], (L, C_Q_LORA, C_HEADS * (C_NOPE + C_ROPE)), C_Q_LORA ** -0.5),
        'mla_kv_norm_g': 1.0 + nrm(ks[15], (L, C_KV_LORA), 0.02),
        'mla_w_ukv': nrm(ks[16], (L, C_KV_LORA, C_HEADS * (C_NOPE + C_V)), C_KV_LORA ** -0.5),
        'w_branch': nrm(ks[17], (L, N_BRANCH, BRANCH_W, D), BRANCH_W ** -0.5),
        'w_out': nrm(ks[18], (L, D, D), D ** -0.5),
        'router_w': nrm(ks[19], (L, D, N_EXPERTS), D ** -0.5),
        'exp_w_gate': nrm(ks[20], (L, N_EXPERTS, D, EXPERT_FF), D ** -0.5),
        'exp_w_up': nrm(ks[21], (L, N_EXPERTS, D, EXPERT_FF), D ** -0.5),
        'exp_w_down': nrm(ks[22], (L, N_EXPERTS, EXPERT_FF, D), EXPERT_FF ** -0.5),
        'final_norm_g': 1.0 + nrm(ks[23], (D,), 0.02),
    }


def reference(x, c, ctx, c_ctx, ada_w, ada_b, norm_mix_g, norm_ffn_g, w_in, hgrn_lb, hgrn_norm_g,
              diff_lambda, diff_norm_g, mla_q_norm_g, mla_w_uq, mla_kv_norm_g, mla_w_ukv, w_branch,
              w_out, router_w, exp_w_gate, exp_w_up, exp_w_down, final_norm_g):
    rows = x.shape[1] // GRID_W
    rope_b = rope_2d(rows, B_HD)
    rope_c = rope_2d(rows, C_ROPE)
    p = jax.nn.softmax(hgrn_lb.astype(F32), axis=0)
    lower_bounds = jnp.clip(jnp.cumsum(p, axis=0) - p[0], 0.0, LB_MAX)
    xc = ctx
    for layer in range(DEPTH):
        with_ctx = layer < DEPTH - 1
        sh_m, sc_m, g_m, sh_f, sc_f, g_f = _modulation(c, ada_w[layer], ada_b[layer])
        csh_m, csc_m, cg_m, csh_f, csc_f, cg_f = _modulation(c_ctx, ada_w[layer], ada_b[layer])
        h_l = _modulate(rmsnorm(x, norm_mix_g[layer]), sh_m, sc_m)
        h_c = _modulate(rmsnorm(xc, norm_mix_g[layer]), csh_m, csc_m)
        mix_l, mix_c = token_mixer(h_l, h_c, layer, lower_bounds[layer], rope_b, rope_c, w_in[layer],
                                   hgrn_norm_g[layer], diff_lambda[layer], diff_norm_g[layer],
                                   mla_q_norm_g[layer], mla_w_uq[layer], mla_kv_norm_g[layer],
                                   mla_w_ukv[layer], w_branch[layer], w_out[layer], with_ctx)
        x = x + g_m * mix_l
        h = _modulate(rmsnorm(x, norm_ffn_g[layer]), sh_f, sc_f)
        x = x + g_f * expert_choice_ffn(h, router_w[layer], exp_w_gate[layer], exp_w_up[layer], exp_w_down[layer])
        if with_ctx:
            xc = xc + cg_m * mix_c
            hc = _modulate(rmsnorm(xc, norm_ffn_g[layer]), csh_f, csc_f)
            xc = xc + cg_f * expert_choice_ffn(hc, router_w[layer], exp_w_gate[layer], exp_w_up[layer], exp_w_down[layer])
    return rmsnorm(x, final_norm_g)
```

```python
import math
import numpy as np
import ml_dtypes
import concourse.bass as bass
import concourse.mybir as mybir
from concourse.bass_utils import run_bass_kernel_spmd

F32 = mybir.dt.float32
BF16 = mybir.dt.bfloat16
ALU = mybir.AluOpType
AF = mybir.ActivationFunctionType

D = 1024
SEQ = 2048
CTX = 256
T = SEQ + CTX
NT = T // 128
DEPTH = 4
N_IN = 7840
NE = 16
FF = 2048
CAP_L = 256
CAP_C = 32
NSLOT = CAP_L + CAP_C
EPS = 1e-6
BLKS = [(0, 256)] + [(256 + 512 * j, 512) for j in range(4)]


class Buf:
    __slots__ = ("lw", "rd", "excl")

    def __init__(self, excl=False):
        self.lw = None
        self.rd = {}
        self.excl = excl


def bufs(n):
    return [Buf() for _ in range(n)]


class Eng:
    def __init__(self, name, sem):
        self.name = name
        self.sem = sem
        self.key = "E" + name
        self.cnt = 0
        self.prog = []
        self.seen = {}
        self.dsems = []
        self.dvals = []
        self.dnext = 0


class Builder:
    def __init__(self, nc, ndma=12):
        self.nc = nc
        self.E = {}
        for n in ("pe", "act", "dve", "pool", "sp"):
            self.E[n] = Eng(n, nc.alloc_semaphore("s_" + n))
        for q in ("sp", "pool", "act"):
            e = self.E[q]
            nd = ndma if q != "act" else 4
            e.dsems = [nc.alloc_semaphore("d_%s%d" % (q, i)) for i in range(nd)]
            e.dvals = [0] * nd
        self.ninstr = 0

    def _deps(self, reads, writes, own=None):
        deps = {}
        for r in reads:
            if r.lw is not None:
                k, s, v = r.lw
                if deps.get(k, (None, -1))[1] < v:
                    deps[k] = (s, v)
            if r.excl:
                for k, (s, v) in r.rd.items():
                    if k != own and deps.get(k, (None, -1))[1] < v:
                        deps[k] = (s, v)
        for w in writes:
            if w.lw is not None:
                k, s, v = w.lw
                if deps.get(k, (None, -1))[1] < v:
                    deps[k] = (s, v)
            for k, (s, v) in w.rd.items():
                if deps.get(k, (None, -1))[1] < v:
                    deps[k] = (s, v)
        return deps

    def _emit_waits(self, eng, deps, skip_self=False):
        for k, (s, v) in deps.items():
            if skip_self and k == eng.key:
                continue
            if eng.seen.get(k, -1) >= v:
                continue
            eng.seen[k] = v
            eng.prog.append(lambda e, s=s, v=v: e.wait_ge(s, v))
            self.ninstr += 1

    def op(self, en, fn, reads=(), writes=(), inc=True):
        eng = self.E[en]
        deps = self._deps(reads, writes, eng.key)
        self._emit_waits(eng, deps, skip_self=(en == "pe"))
        val = eng.cnt + 1
        if inc:
            eng.cnt = val
            sem = eng.sem
            eng.prog.append(lambda e, fn=fn, sem=sem: fn(e).then_inc(sem, 1))
        else:
            assert en == "pe"
            eng.prog.append(lambda e, fn=fn: fn(e))
        self.ninstr += 1
        tag = (eng.key, eng.sem, val)
        for w in writes:
            w.lw = tag
            w.rd = {}
        for r in reads:
            r.rd[eng.key] = (eng.sem, val)

    def dma(self, qn, out, in_, reads=(), writes=(), **kw):
        eng = self.E[qn]
        deps = self._deps(reads, writes)
        i = eng.dnext
        eng.dnext = (i + 1) % len(eng.dsems)
        sem = eng.dsems[i]
        key = "D%s%d" % (qn, i)
        if eng.dvals[i] > 0:
            if deps.get(key, (None, -1))[1] < eng.dvals[i]:
                deps[key] = (sem, eng.dvals[i])
        self._emit_waits(eng, deps)
        eng.dvals[i] += 16
        val = eng.dvals[i]
        eng.prog.append(lambda e, out=out, in_=in_, sem=sem, kw=kw:
                        e.dma_start(out=out, in_=in_, **kw).then_inc(sem, 16))
        self.ninstr += 1
        tag = (key, sem, val)
        for w in writes:
            w.lw = tag
            w.rd = {}
        for r in reads:
            r.rd[key] = (sem, val)
        return tag

    def barrier(self):
        deps = {}
        for e in self.E.values():
            if e.cnt > 0:
                deps[e.key] = (e.sem, e.cnt)
            for i, s in enumerate(e.dsems):
                if e.dvals[i] > 0:
                    deps["D%s%d" % (e.name, i)] = (s, e.dvals[i])
        for e in self.E.values():
            self._emit_waits(e, dict(deps))

    def finish(self):
        self.barrier()
        nc = self.nc
        E = self.E
        with nc.Block() as block:
            @block.tensor
            def _(e):
                for f in E["pe"].prog:
                    f(e)

            @block.scalar
            def _(e):
                for f in E["act"].prog:
                    f(e)

            @block.vector
            def _(e):
                for f in E["dve"].prog:
                    f(e)

            @block.gpsimd
            def _(e):
                for f in E["pool"].prog:
                    f(e)

            @block.sync
            def _(e):
                for f in E["sp"].prog:
                    f(e)


def _rope_tables():
    rows = SEQ // 64
    row = np.repeat(np.arange(rows, dtype=np.float32), 64)
    col = np.tile(np.arange(64, dtype=np.float32), rows)

    def ang(rot_dim):
        nf = rot_dim // 4
        inv = (10000.0 ** (-np.arange(nf, dtype=np.float32) / nf)).astype(np.float32)
        return np.concatenate([row[:, None] * inv, col[:, None] * inv], axis=-1)

    ab = ang(64)
    ac = ang(32)
    cb, sb = np.cos(ab).astype(np.float32), np.sin(ab).astype(np.float32)
    cc, sc = np.cos(ac).astype(np.float32), np.sin(ac).astype(np.float32)
    Cb = np.zeros((128, SEQ), np.float32)
    Sb = np.zeros((128, SEQ), np.float32)
    for p in range(128):
        j = p % 64
        Cb[p] = cb[:, j % 32]
        Sb[p] = sb[:, j % 32]
    Cc = np.ones((128, SEQ), np.float32)
    Sc = np.zeros((128, SEQ), np.float32)
    for p in range(64, 96):
        Cc[p] = cc[:, (p - 64) % 16]
        Sc[p] = sc[:, (p - 64) % 16]
    Pb = np.zeros((128, 128), np.float32)
    for m in range(2):
        for i in range(32):
            Pb[m * 64 + i + 32, m * 64 + i] = -1.0
            Pb[m * 64 + i, m * 64 + i + 32] = 1.0
    Pc = np.zeros((128, 128), np.float32)
    for i in range(16):
        Pc[64 + i + 16, 64 + i] = -1.0
        Pc[64 + i, 64 + i + 16] = 1.0
    return Cb, Sb, Cc, Sc, Pb, Pc


def _consts():
    Cb, Sb, Cc, Sc, Pb, Pc = _rope_tables()
    s = np.arange(128)[:, None]
    t = np.arange(128)[None, :]
    same = (s // 32) == (t // 32)
    mc = (same & (s <= t)).astype(np.float32)
    ma = (same & (s >= t)).astype(np.float32)
    bf = ml_dtypes.bfloat16
    iota_c = np.tile(np.arange(NSLOT, dtype=np.float32)[None, :], (128, 1))
    iota_p = np.stack([np.arange(128, dtype=np.float32) + 128 * k for k in range(3)], axis=1)
    pidx16 = np.tile(np.arange(16, dtype=np.float32)[:, None], (1, 128))
    return dict(ident_f=np.eye(128, dtype=np.float32), ident_b=np.eye(128).astype(bf),
                ropeCb=Cb, ropeSb=Sb, ropeCc=Cc, ropeSc=Sc, Pb=Pb.astype(bf), Pc=Pc.astype(bf),
                maskc=mc.astype(bf), maska=ma.astype(bf), iota_c=iota_c, iota_p=iota_p,
                pidx16=pidx16)


CONST_SPECS = dict(ident_f=([128, 128], F32), ident_b=([128, 128], BF16),
                   ropeCb=([128, SEQ], F32), ropeSb=([128, SEQ], F32), ropeCc=([128, SEQ], F32),
                   ropeSc=([128, SEQ], F32), Pb=([128, 128], BF16), Pc=([128, 128], BF16),
                   maskc=([128, 128], BF16), maska=([128, 128], BF16),
                   iota_c=([128, NSLOT], F32), iota_p=([128, 3], F32), pidx16=([16, 128], F32))

def weight_specs(NLW):
    return dict(ada_w=[NLW, D, 6 * D], ada_b=[NLW, 6 * D], norm_mix_g=[NLW, D], norm_ffn_g=[NLW, D],
                    w_in=[NLW, D, N_IN], diff_lambda=[NLW, 256],
                    mla_w_uq=[NLW, 384, 768], mla_w_ukv=[NLW, 256, 1024],
                    w_branch=[NLW, 3, 512, D], w_out=[NLW, D, D], router_w=[NLW, D, NE],
                    exp_w_gate=[NLW, NE, D, FF], exp_w_up=[NLW, NE, D, FF], exp_w_down=[NLW, NE, FF, D],
                    final_norm_g=[1, D],
                    hlbT=[128, DEPTH * 8], hngT=[128, DEPTH * 4], dngT=[128, DEPTH * 4],
                    qngT=[128, DEPTH * 3], kvngT=[128, DEPTH * 2])


def build(NB, NL, dbg=False, stop_after=None, NLW=DEPTH, dbg_layer=0, l0=0, chain=False, do_final=True):
    nc = bass.Bass("TRN2", target_bir_lowering=False)
    B = Builder(nc)
    dr = {}

    def din(name, shape, dt=F32):
        dr[name] = nc.dram_tensor(name, list(shape), dt, kind="ExternalInput").ap()
        return dr[name]

    x_in = din("x", [NB, SEQ, D])
    ctx_in = din("ctx", [NB, CTX, D])
    NRP = NB + 1 + ((NB + 1) % 2)
    cT_in = din("cT", [128, 8 * NRP])
    for k, shp in weight_specs(NLW).items():
        din(k, shp)
    for k, (shp, dt) in CONST_SPECS.items():
        din(k, shp, dt)
    out_d = nc.dram_tensor("out", [NB, SEQ, D], F32, kind="ExternalOutput").ap()
    if chain:
        xo_d = nc.dram_tensor("x_out", [NB, SEQ, D], F32, kind="ExternalOutput").ap()
        co_d = nc.dram_tensor("ctx_out", [NB, CTX, D], F32, kind="ExternalOutput").ap()
    skind = "ExternalOutput" if dbg else "Internal"
    NR = NB + 1
    modv = nc.dram_tensor("modv", [NL, NR, 6, D], F32, kind=skind).ap()
    br_d = nc.dram_tensor("br_d", [3, 512, T], BF16, kind=skind).ap()
    mrg_d = nc.dram_tensor("mrg_d", [D, T], BF16, kind=skind).ap()
    dbg_d = {}

    def dbg_out(name, shape, dt=F32):
        dbg_d[name] = nc.dram_tensor("dbg_" + name, list(shape), dt, kind="ExternalOutput").ap()
        return dbg_d[name]

    xs = nc.alloc_sbuf_tensor("xs", [128, NT, D], F32)
    bx = bufs(NT)
    csb = {}
    for k, (shp, dt) in CONST_SPECS.items():
        if k.startswith("rope"):
            continue
        csb[k] = nc.alloc_sbuf_tensor("c_" + k, shp, dt)
    ones_f = nc.alloc_sbuf_tensor("ones_f", [128, 128], F32)
    ones_b = nc.alloc_sbuf_tensor("ones_b", [128, 128], BF16)
    small = nc.alloc_sbuf_tensor("small", [128, 384], F32)
    bsmall = Buf()
    bconst = Buf()
    lbs_all = nc.alloc_sbuf_tensor("lbs_all", [128, DEPTH * 8], F32)
    hparams = nc.alloc_sbuf_tensor("hparams", [128, DEPTH * 13], F32)
    ARENA_F32 = nc.sbuf_bytes_remaining // 4 - 64
    arena = nc.alloc_sbuf_tensor("arena", [128, ARENA_F32], F32)
    ps = [nc.alloc_psum_tensor("ps%d" % i, [128, 512], F32) for i in range(8)]
    bp = [Buf(excl=True) for _ in range(8)]

    class Arena:
        def __init__(self, base=0):
            self.off = base

        def f32(self, n, parts=128):
            a = arena[0:parts, self.off:self.off + n]
            self.off += n
            assert self.off <= ARENA_F32, ("arena overflow", self.off, ARENA_F32)
            return a

        def bf(self, n, parts=128):
            w = (n + 1) // 2
            a = arena[0:parts, self.off:self.off + w].bitcast(BF16)
            self.off += w
            assert self.off <= ARENA_F32, ("arena overflow", self.off, ARENA_F32)
            return a[:, 0:n]

    A0 = Arena()
    hT = A0.bf(8 * T).rearrange("p (c t) -> p c t", c=8)
    bh = bufs(NT)
    MIX_BASE = A0.off

    def tiles_of(t0, n):
        return list(range(t0 // 128, (t0 + n + 127) // 128))

    for k in csb:
        B.dma("sp", csb[k][:], dr[k], writes=[bconst])
    B.op("dve", lambda e: e.memset(ones_f[:], 1.0), writes=[bconst])
    B.op("dve", lambda e: e.memset(ones_b[:], 1.0), writes=[bconst])
    ident_f, ident_b = csb["ident_f"], csb["ident_b"]

    bhp = Buf()
    B.dma("sp", hparams[:, 0:16], dr["hngT"], writes=[bhp])
    B.dma("sp", hparams[:, 16:32], dr["dngT"], writes=[bhp])
    B.dma("sp", hparams[:, 32:44], dr["qngT"], writes=[bhp])
    B.dma("sp", hparams[:, 44:52], dr["kvngT"], writes=[bhp])

    def lower_bounds():
        a = Arena(MIX_BASE)
        raw = a.f32(32)
        ex = a.f32(32)
        sm = a.f32(8)
        b0 = Buf()
        B.dma("sp", raw, dr["hlbT"], writes=[b0])
        B.op("act", lambda e: e.activation(out=ex, in_=raw, func=AF.Exp), reads=[b0], writes=[b0])
        exv = ex.rearrange("p (l k) -> p l k", l=DEPTH)
        B.op("dve", lambda e: e.tensor_tensor(out=sm, in0=exv[:, 0, :], in1=exv[:, 1, :], op=ALU.add), reads=[b0], writes=[b0])
        B.op("dve", lambda e: e.tensor_tensor(out=sm, in0=sm, in1=exv[:, 2, :], op=ALU.add), reads=[b0], writes=[b0])
        B.op("dve", lambda e: e.tensor_tensor(out=sm, in0=sm, in1=exv[:, 3, :], op=ALU.add), reads=[b0], writes=[b0])
        B.op("dve", lambda e: e.reciprocal(out=sm, in_=sm), reads=[b0], writes=[b0])
        lb = lbs_all[:, :].rearrange("p (l k) -> p l k", l=DEPTH)
        B.op("dve", lambda e: e.memset(lb[:, 0, :], 0.0), writes=[b0])
        for l in range(1, DEPTH):
            B.op("dve", lambda e, l=l: e.tensor_tensor(out=raw[:, 0:8], in0=exv[:, l, :], in1=sm, op=ALU.mult), reads=[b0], writes=[b0])
            B.op("dve", lambda e, l=l: e.tensor_tensor(out=lb[:, l, :], in0=lb[:, l - 1, :], in1=raw[:, 0:8], op=ALU.add), reads=[b0], writes=[b0])
        B.op("dve", lambda e: e.tensor_scalar(out=lbs_all[:, :], in0=lbs_all[:, :], scalar1=0.0, scalar2=1.0 - 1e-6,
                                              op0=ALU.max, op1=ALU.min), reads=[b0], writes=[b0, bhp])

    lower_bounds()
    B.barrier()

    def modulation():
        a = Arena(MIX_BASE)
        cT = a.f32(8 * NRP)
        scT = a.f32(8 * NRP)
        awt = [a.f32(3072) for _ in range(2)]
        baw = bufs(2)
        rows = a.f32(6 * D, parts=NRP)
        brow = Buf()
        gb = a.f32(2 * D, parts=NRP)
        bgb = Buf()
        bc = Buf()
        B.dma("sp", cT, cT_in, writes=[bc])
        B.op("act", lambda e: e.activation(out=scT, in_=cT, func=AF.Sigmoid), reads=[bc], writes=[bc])
        B.op("dve", lambda e: e.tensor_tensor(out=scT, in0=scT, in1=cT, op=ALU.mult), reads=[bc], writes=[bc])
        scv = scT.rearrange("p (c r) -> p c r", c=8)
        it = 0
        for l in range(NL):
            B.dma("sp", rows, dr["ada_b"][l:l + 1, :].to_broadcast([NRP, 6 * D]), writes=[brow])
            B.dma("sp", gb[:, 0:D], dr["norm_mix_g"][l:l + 1, :].to_broadcast([NRP, D]), writes=[bgb])
            B.dma("sp", gb[:, D:2 * D], dr["norm_ffn_g"][l:l + 1, :].to_broadcast([NRP, D]), writes=[bgb])
            for half in range(2):
                for k in range(8):
                    j = it % 2
                    it += 1
                    B.dma("sp", awt[j], dr["ada_w"][l, k * 128:(k + 1) * 128, half * 3072:(half + 1) * 3072], writes=[baw[j]])
                    for q in range(6):
                        B.op("pe", lambda e, k=k, j=j, q=q: e.matmul(ps[q][0:NRP, :], lhsT=scv[:, k, :], rhs=awt[j][:, q * 512:(q + 1) * 512],
                                                                    start=(k == 0), stop=(k == 7)),
                             reads=[bc, baw[j]], writes=[bp[q]], inc=(q == 5))
                for q in range(6):
                    c0 = half * 3072 + q * 512
                    B.op("dve", lambda e, q=q, c0=c0: e.tensor_tensor(out=rows[:, c0:c0 + 512], in0=ps[q][0:NRP, :], in1=rows[:, c0:c0 + 512], op=ALU.add),
                         reads=[bp[q], brow], writes=[brow])
            B.op("dve", lambda e: e.scalar_tensor_tensor(out=rows[:, D:2 * D], in0=rows[:, D:2 * D], scalar=1.0, in1=gb[:, 0:D], op0=ALU.add, op1=ALU.mult),
                 reads=[brow, bgb], writes=[brow])
            B.op("dve", lambda e: e.scalar_tensor_tensor(out=rows[:, 4 * D:5 * D], in0=rows[:, 4 * D:5 * D], scalar=1.0, in1=gb[:, D:2 * D], op0=ALU.add, op1=ALU.mult),
                 reads=[brow, bgb], writes=[brow])
            rv = rows.rearrange("p (s d) -> p s d", s=6)
            for dst, src in enumerate((1, 0, 2, 4, 3, 5)):
                B.dma("sp", modv[l, :, dst, :], rv[0:NR, src, :], reads=[brow])

    modulation()
    B.barrier()

    def load_x(b):
        for i in range(NT):
            if i < 2:
                src = ctx_in[b, i * 128:(i + 1) * 128, :]
            else:
                src = x_in[b, (i - 2) * 128:(i - 1) * 128, :]
            B.dma("sp", xs[:, i, :], src, writes=[bx[i]])

    def layer_small(b, l_):
        import os as _os
        l = l_ + l0 + int(_os.environ.get("LOFF", "0"))
        sv = small
        B.op("dve", lambda e: e.tensor_copy(out=sv[:, 0:8], in_=lbs_all[:, l * 8:(l + 1) * 8]), reads=[bhp], writes=[bsmall])
        B.op("dve", lambda e: e.tensor_copy(out=sv[:, 8:12], in_=hparams[:, l * 4:(l + 1) * 4]), reads=[bhp], writes=[bsmall])
        B.op("dve", lambda e: e.tensor_copy(out=sv[:, 12:16], in_=hparams[:, 16 + l * 4:16 + (l + 1) * 4]), reads=[bhp], writes=[bsmall])
        B.op("dve", lambda e: e.tensor_copy(out=sv[:, 16:19], in_=hparams[:, 32 + l * 3:32 + (l + 1) * 3]), reads=[bhp], writes=[bsmall])
        B.op("dve", lambda e: e.tensor_copy(out=sv[:, 19:21], in_=hparams[:, 44 + l * 2:44 + (l + 1) * 2]), reads=[bhp], writes=[bsmall])
        B.op("dve", lambda e: e.tensor_scalar(out=sv[:, 56:64], in0=sv[:, 0:8], scalar1=-1.0, scalar2=1.0, op0=ALU.mult, op1=ALU.add),
             reads=[bsmall], writes=[bsmall])
        lam_init = 0.8 - 0.6 * math.exp(-0.3 * l)
        dl = sv[:, 64:64 + 256]
        B.dma("sp", dl, dr["diff_lambda"][l_:l_ + 1, :].to_broadcast([128, 256]), writes=[bsmall])
        B.op("dve", lambda e: e.tensor_tensor(out=dl[:, 0:64], in0=dl[:, 0:64], in1=dl[:, 64:128], op=ALU.mult), reads=[bsmall], writes=[bsmall])
        B.op("dve", lambda e: e.tensor_tensor(out=dl[:, 128:192], in0=dl[:, 128:192], in1=dl[:, 192:256], op=ALU.mult), reads=[bsmall], writes=[bsmall])
        B.op("dve", lambda e: e.reduce_sum(out=sv[:, 22:23], in_=dl[:, 0:64], axis=mybir.AxisListType.X), reads=[bsmall], writes=[bsmall])
        B.op("dve", lambda e: e.reduce_sum(out=sv[:, 23:24], in_=dl[:, 128:192], axis=mybir.AxisListType.X), reads=[bsmall], writes=[bsmall])
        B.op("act", lambda e: e.activation(out=sv[:, 22:24], in_=sv[:, 22:24], func=AF.Exp), reads=[bsmall], writes=[bsmall])
        B.op("dve", lambda e: e.tensor_tensor(out=sv[:, 21:22], in0=sv[:, 23:24], in1=sv[:, 22:23], op=ALU.subtract), reads=[bsmall], writes=[bsmall])
        B.op("dve", lambda e: e.tensor_scalar(out=sv[:, 21:22], in0=sv[:, 21:22], scalar1=-lam_init, scalar2=None, op0=ALU.add), reads=[bsmall], writes=[bsmall])
        for j, (r, which) in enumerate(((b, 0), (b, 1), (NB, 0), (NB, 1))):
            B.dma("sp", sv[:, 24 + 8 * j:32 + 8 * j], modv[l_, r, which, :].rearrange("(c p) -> p c", p=128), writes=[bsmall],
                  allow_slow_non_contiguous=True)

    def rows_of(i, b):
        return NB if i < 2 else b

    def norm1(b, l):
        a = Arena(MIX_BASE)
        ssq = a.f32(NT)
        rstd = a.f32(NT)
        junk = a.bf(D)
        xn = [a.f32(D) for _ in range(2)]
        bss, brs, bj = Buf(), Buf(), Buf()
        bxn = bufs(2)
        for i in range(NT):
            B.op("act", lambda e, i=i: e.activation(out=junk, in_=xs[:, i, :], func=AF.Square, accum_out=ssq[:, i:i + 1]),
                 reads=[bx[i]], writes=[bj, bss])
        B.op("act", lambda e: e.activation(out=rstd, in_=ssq, func=AF.Sqrt, scale=1.0 / D, bias=EPS), reads=[bss], writes=[brs])
        B.op("dve", lambda e: e.reciprocal(out=rstd, in_=rstd), reads=[brs], writes=[brs])
        for i in range(NT):
            j = i % 2
            B.op("dve", lambda e, i=i, j=j: e.tensor_scalar(out=xn[j], in0=xs[:, i, :], scalar1=rstd[:, i:i + 1], scalar2=None, op0=ALU.mult),
                 reads=[bx[i], brs], writes=[bxn[j]])
            for c in range(8):
                bk = 2 * j + c // 4
                B.op("pe", lambda e, c=c, j=j, bk=bk: e.transpose(out=ps[bk][:, (c % 4) * 128:(c % 4 + 1) * 128], in_=xn[j][:, c * 128:(c + 1) * 128], identity=ident_f[:]),
                     reads=[bxn[j], bconst], writes=[bp[bk]], inc=(c % 4 == 3))
            co = 24 if i >= 2 else 40
            for c in range(8):
                bk = 2 * j + c // 4
                src = ps[bk][:, (c % 4) * 128:(c % 4 + 1) * 128]
                dst = hT[:, c, i * 128:(i + 1) * 128]
                if c < 4:
                    B.op("act", lambda e, src=src, dst=dst, c=c, co=co: e.activation(out=dst, in_=src, func=AF.Identity, scale=small[:, co + c:co + c + 1], bias=small[:, co + 8 + c:co + 9 + c]),
                         reads=[bp[bk], bsmall], writes=[bh[i]])
                else:
                    B.op("dve", lambda e, src=src, dst=dst, c=c, co=co: e.tensor_scalar(out=dst, in0=src, scalar1=small[:, co + c:co + c + 1], scalar2=small[:, co + 8 + c:co + 9 + c], op0=ALU.mult, op1=ALU.add),
                         reads=[bp[bk], bsmall], writes=[bh[i]])

    def load_w(dst, src_ap, bw, q="pool"):
        B.dma(q, dst, src_ap.rearrange("(c p) n -> p c n", p=128), writes=[bw])

    def proj_fm(bank, w_sb, wcols, kchunks, src, bsrc_of_tile, t0, n, bw, M=128, out_part0=0, tile_pos=None, start_group=True):
        rd = [bw] + [bsrc_of_tile[i] for i in tiles_of(t0, n)]
        for c in range(kchunks):
            kw = {}
            if tile_pos is not None:
                kw["tile_position"] = tile_pos
            B.op("pe", lambda e, c=c, kw=kw: e.matmul(ps[bank][out_part0:out_part0 + M, 0:n], lhsT=w_sb[:, c, wcols[0]:wcols[1]], rhs=src[:, c, t0:t0 + n],
                                                      start=(c == 0), stop=(c == kchunks - 1), **kw),
                 reads=rd, writes=[bp[bank]], inc=(c == kchunks - 1))

    def colnorm(bank_ss, sq_src_list, n, scale, out_rstd, b_out, breads):
        for k, (sq, K) in enumerate(sq_src_list):
            B.op("pe", lambda e, k=k, sq=sq, K=K: e.matmul(ps[bank_ss][:, 0:n], lhsT=ones_f[0:K, :], rhs=sq, start=(k == 0), stop=(k == len(sq_src_list) - 1)),
                 reads=breads + [bconst], writes=[bp[bank_ss]], inc=(k == len(sq_src_list) - 1))
        B.op("act", lambda e: e.activation(out=out_rstd, in_=ps[bank_ss][:, 0:n], func=AF.Sqrt, scale=scale, bias=EPS), reads=[bp[bank_ss]], writes=[b_out])
        B.op("dve", lambda e: e.reciprocal(out=out_rstd, in_=out_rstd), reads=[b_out], writes=[b_out])

    def hgrn(b, l):
        a = Arena(MIX_BASE)
        w5 = a.bf(8 * 640).rearrange("p (c n) -> p c n", c=8)
        bw5 = Buf()
        qT = a.bf(T)
        gT = a.bf(T)
        vtok = a.bf(NT * 128).rearrange("p (i v) -> p i v", i=NT)
        lf = a.f32(T)
        bcs = a.f32(T)
        kk = a.bf(T)
        tmpE = a.bf(T)
        qtl = a.bf(T)
        ktl = a.bf(T)
        khat = a.bf(T)
        dec = a.f32(72)
        oT = a.f32(T)
        S = [a.f32(128) for _ in range(2)]
        Sb = [a.bf(128) for _ in range(2)]
        msk = [a.bf(128) for _ in range(2)]
        khT = [a.bf(128) for _ in range(2)]
        aout = a.bf(T)
        ones72 = a.bf(T)
        bq, bg, bv, blf, bbc, bkk, btE, bqt, bkt, bkh, bdec, bo, bao, b1 = [Buf() for _ in range(14)]
        bS = bufs(2)
        bSb = bufs(2)
        bm = bufs(2)
        bkT = bufs(2)
        B.op("pool", lambda e: e.memset(ones72, 1.0), writes=[b1])
        B.op("pool", lambda e: e.memset(ones72.rearrange("p (c s) -> p c s", s=32)[:, :, 0:1], 0.0), writes=[b1])
        for h in range(4):
            for g in range(5):
                load_w(w5[:, :, g * 128:(g + 1) * 128], dr["w_in"][l, :, g * 512 + h * 128:g * 512 + (h + 1) * 128], bw5)
            for bi, (t0, n) in enumerate(BLKS):
                proj_fm(bi % 2, w5, (0, 128), 8, hT, bh, t0, n, bw5)
                B.op("act", lambda e, t0=t0, n=n, bi=bi: e.activation(out=qT[:, t0:t0 + n], in_=ps[bi % 2][:, 0:n], func=AF.Copy), reads=[bp[bi % 2]], writes=[bq])
                proj_fm(2 + bi % 2, w5, (256, 384), 8, hT, bh, t0, n, bw5)
                B.op("act", lambda e, t0=t0, n=n, bi=bi: e.activation(out=gT[:, t0:t0 + n], in_=ps[2 + bi % 2][:, 0:n], func=AF.Silu), reads=[bp[2 + bi % 2]], writes=[bg])
            for i in range(NT):
                bk = 4 + i % 2
                for c in range(8):
                    B.op("pe", lambda e, i=i, c=c, bk=bk: e.matmul(ps[bk][:, 0:128], lhsT=hT[:, c, i * 128:(i + 1) * 128], rhs=w5[:, c, 128:256], start=(c == 0), stop=(c == 7)),
                         reads=[bh[i], bw5], writes=[bp[bk]], inc=(c == 7))
                B.op("dve", lambda e, i=i, bk=bk: e.tensor_copy(out=vtok[:, i, :], in_=ps[bk][:, 0:128]), reads=[bp[bk]], writes=[bv])
            import os as _os
            for dr_ in range(int(_os.environ.get('DBG_NDIR', '2'))):
                if _os.environ.get("DBG_BAR") == "1":
                    B.barrier()
                lbc = small[:, dr_ * 4 + h:dr_ * 4 + h + 1]
                omlb = small[:, 56 + dr_ * 4 + h:56 + dr_ * 4 + h + 1]
                for bi, (t0, n) in enumerate(BLKS):
                    bk = 6 + bi % 2
                    proj_fm(bk, w5, (384 + dr_ * 128, 512 + dr_ * 128), 8, hT, bh, t0, n, bw5)
                    B.op("act", lambda e, t0=t0, n=n, bk=bk: e.activation(out=lf[:, t0:t0 + n], in_=ps[bk][:, 0:n], func=AF.Sigmoid), reads=[bp[bk]], writes=[blf])
                B.op("dve", lambda e, omlb=omlb, lbc=lbc: e.tensor_scalar(out=lf, in0=lf, scalar1=omlb, scalar2=lbc, op0=ALU.mult, op1=ALU.add), reads=[blf, bsmall], writes=[blf])
                B.op("dve", lambda e: e.tensor_scalar(out=kk, in0=lf, scalar1=-1.0, scalar2=1.0, op0=ALU.mult, op1=ALU.add), reads=[blf], writes=[bkk])
                B.op("act", lambda e: e.activation(out=lf, in_=lf, func=AF.Ln), reads=[blf], writes=[blf])
                B.op("dve", lambda e: e.tensor_tensor_scan(out=bcs, data0=ones72, data1=lf, initial=0.0, op0=ALU.mult, op1=ALU.add), reads=[blf, b1], writes=[bbc])
                bc3 = bcs.rearrange("p (c s) -> p c s", s=32)
                B.op("act", lambda e: e.activation(out=dec.rearrange("p (c o) -> p c o", o=1), in_=bc3[:, :, 31:32], func=AF.Exp), reads=[bbc], writes=[bdec])
                if dr_ == 1:
                    B.op("dve", lambda e: e.tensor_tensor(out=bcs, in0=lf, in1=bcs, op=ALU.subtract), reads=[blf, bbc], writes=[bbc])
                    B.op("dve", lambda e: e.tensor_reduce(out=dec.rearrange("p (c o) -> p c o", o=1), in_=lf.rearrange("p (c s) -> p c s", s=32), axis=mybir.AxisListType.X, op=ALU.add),
                         reads=[blf, bdec], writes=[bdec])
                    B.op("dve", lambda e: e.tensor_tensor(out=bc3, in0=bc3, in1=dec.unsqueeze(2).to_broadcast([128, 72, 32]), op=ALU.add), reads=[bbc, bdec], writes=[bbc])
                    B.op("act", lambda e: e.activation(out=dec, in_=dec, func=AF.Exp), reads=[bdec], writes=[bdec])
                B.op("act", lambda e: e.activation(out=tmpE, in_=bcs, func=AF.Exp), reads=[bbc], writes=[btE])
                B.op("dve", lambda e: e.tensor_tensor(out=qtl, in0=qT, in1=tmpE, op=ALU.mult), reads=[bq, btE], writes=[bqt])
                B.op("act", lambda e: e.activation(out=tmpE, in_=bcs, func=AF.Exp, scale=-1.0), reads=[bbc, bqt], writes=[btE])
                B.op("dve", lambda e: e.tensor_tensor(out=ktl, in0=kk, in1=tmpE, op=ALU.mult), reads=[bkk, btE], writes=[bkt])
                B.op("dve", lambda e: e.tensor_tensor(out=khat.rearrange("p (c s) -> p c s", s=32), in0=ktl.rearrange("p (c s) -> p c s", s=32),
                                                      in1=dec.unsqueeze(2).to_broadcast([128, 72, 32]), op=ALU.mult), reads=[bkt, bdec], writes=[bkh])
                B.op("pool", lambda e: e.memset(S[0], 0.0), writes=[bS[0]])
                B.op("pool", lambda e: e.memset(Sb[0], 0.0), writes=[bSb[0]])
                cur = 0
                order = list(range(NT)) if dr_ == 0 else [1, 0] + list(range(NT - 1, 1, -1))
                mask = csb["maskc"] if dr_ == 0 else csb["maska"]
                for ti, i in enumerate(order):
                    if dr_ == 1 and _os.environ.get("DBG_BWD") == "noscan":
                        break
                    j = ti % 2
                    tsl = slice(i * 128, (i + 1) * 128)
                    B.op("pe", lambda e, tsl=tsl, j=j: e.matmul(ps[j][:, 0:128], lhsT=ktl[:, tsl], rhs=qtl[:, tsl], start=True, stop=True),
                         reads=[bkt, bqt], writes=[bp[j]])
                    B.op("dve", lambda e, j=j, mask=mask: e.tensor_tensor(out=msk[j], in0=ps[j][:, 0:128], in1=mask[:], op=ALU.mult), reads=[bp[j], bconst], writes=[bm[j]])
                    pkt = ps[2 + j][:, 0:64].bitcast(BF16)
                    B.op("pe", lambda e, tsl=tsl, pkt=pkt: e.transpose(out=pkt, in_=khat[:, tsl], identity=ident_b[:]), reads=[bkh, bconst], writes=[bp[2 + j]])
                    B.op("act", lambda e, j=j, pkt=pkt: e.activation(out=khT[j], in_=pkt, func=AF.Copy), reads=[bp[2 + j]], writes=[bkT[j]])
                    _ni = (dr_ == 1 and _os.environ.get("DBG_BWD") == "nointer")
                    B.op("pe", lambda e, i=i, j=j, _ni=_ni: e.matmul(ps[4 + j][:, 0:128], lhsT=vtok[:, i, :], rhs=msk[j], start=True, stop=_ni),
                         reads=[bv, bm[j]], writes=[bp[4 + j]], inc=_ni)
                    corder = range(4) if dr_ == 0 else range(3, -1, -1)
                    for ci, cc in enumerate(corder):
                        csl = slice(i * 128 + cc * 32, i * 128 + cc * 32 + 32)
                        if not (dr_ == 1 and _os.environ.get("DBG_BWD") == "nointer"):
                            B.op("pe", lambda e, j=j, cc=cc, csl=csl, cur=cur, ci=ci: e.matmul(ps[4 + j][:, cc * 32:cc * 32 + 32], lhsT=Sb[cur], rhs=qtl[:, csl], start=False, stop=(ci == 3)),
                                 reads=[bSb[cur], bqt], writes=[bp[4 + j]], inc=(ci == 3))
                        bk = 6 + (ci % 2)
                        B.op("pe", lambda e, j=j, cc=cc, i=i, bk=bk: e.matmul(ps[bk][:, 0:128], lhsT=khT[j][cc * 32:cc * 32 + 32, :], rhs=vtok[cc * 32:cc * 32 + 32, i, :], start=True, stop=True,
                                                                               tile_position=(cc * 32, 0)),
                             reads=[bkT[j], bv], writes=[bp[bk]])
                        nxt = 1 - cur
                        chunk = i * 4 + cc
                        B.op("dve", lambda e, cur=cur, nxt=nxt, bk=bk, chunk=chunk: e.scalar_tensor_tensor(out=S[nxt], in0=S[cur], scalar=dec[:, chunk:chunk + 1], in1=ps[bk][:, 0:128], op0=ALU.mult, op1=ALU.add),
                             reads=[bS[cur], bdec, bp[bk]], writes=[bS[nxt]])
                        B.op("dve", lambda e, cur=cur, nxt=nxt, bk=bk, chunk=chunk: e.scalar_tensor_tensor(out=Sb[nxt], in0=S[cur], scalar=dec[:, chunk:chunk + 1], in1=ps[bk][:, 0:128], op0=ALU.mult, op1=ALU.add),
                             reads=[bS[cur], bdec, bp[bk]], writes=[bSb[nxt]])
                        cur = nxt
                    if dr_ == 0:
                        B.op("act", lambda e, tsl=tsl, j=j: e.activation(out=oT[:, tsl], in_=ps[4 + j][:, 0:128], func=AF.Copy), reads=[bp[4 + j]], writes=[bo])
                    elif _os.environ.get("DBG_BWD") != "noadd":
                        B.op("dve", lambda e, tsl=tsl, j=j: e.tensor_tensor(out=oT[:, tsl], in0=ps[4 + j][:, 0:128], in1=oT[:, tsl], op=ALU.add), reads=[bp[4 + j], bo], writes=[bo])
            for bi, (t0, n) in enumerate(BLKS):
                sq = lf[:, t0:t0 + n]
                B.op("act", lambda e, sq=sq, t0=t0, n=n: e.activation(out=sq, in_=oT[:, t0:t0 + n], func=AF.Square), reads=[bo, blf], writes=[blf])
                rst = bcs[:, t0:t0 + n]
                colnorm(bi % 2, [(sq, 128)], n, 1.0 / 128, rst, bbc, [blf])
                B.op("dve", lambda e, t0=t0, n=n, rst=rst: e.tensor_tensor(out=rst, in0=oT[:, t0:t0 + n], in1=rst, op=ALU.mult), reads=[bo, bbc], writes=[bbc])
                B.op("dve", lambda e, t0=t0, n=n, rst=rst, h=h: e.scalar_tensor_tensor(out=aout[:, t0:t0 + n], in0=rst, scalar=small[:, 8 + h:9 + h], in1=gT[:, t0:t0 + n], op0=ALU.mult, op1=ALU.mult),
                     reads=[bbc, bg, bsmall], writes=[bao])
            B.dma("sp", br_d[0, h * 128:(h + 1) * 128, :], aout, reads=[bao])

    def attention(qT_, kT_, Kdim, kbase, vtok_, vcols, scale, qt0, qn, ktiles, bank_o, bank_l, out_part0, M, pt, bpt, bq_, bk_, bv_):
        nk = len(ktiles)
        tp = (0, out_part0) if out_part0 else None
        for ki, kt in enumerate(ktiles):
            sb_ = ki % 3
            B.op("pe", lambda e, kt=kt, sb_=sb_: e.matmul(ps[sb_][:, 0:qn], lhsT=kT_[kbase:kbase + Kdim, kt * 128:(kt + 1) * 128], rhs=qT_[kbase:kbase + Kdim, qt0:qt0 + qn], start=True, stop=True),
                 reads=[bk_, bq_], writes=[bp[sb_]])
            B.op("act", lambda e, sb_=sb_: e.activation(out=pt[sb_][:, 0:qn], in_=ps[sb_][:, 0:qn], func=AF.Exp, scale=scale), reads=[bp[sb_]], writes=[bpt[sb_]])
            kw = {"tile_position": tp} if tp else {}
            B.op("pe", lambda e, kt=kt, sb_=sb_, ki=ki, kw=kw: e.matmul(ps[bank_o][out_part0:out_part0 + M, 0:qn], lhsT=vtok_[:, kt, vcols[0]:vcols[1]], rhs=pt[sb_][:, 0:qn],
                                                                         start=(ki == 0), stop=(ki == nk - 1), **kw),
                 reads=[bv_, bpt[sb_]], writes=[bp[bank_o]], inc=False)
            B.op("pe", lambda e, sb_=sb_, ki=ki, kw=kw: e.matmul(ps[bank_l][out_part0:out_part0 + M, 0:qn], lhsT=ones_b[:, 0:M], rhs=pt[sb_][:, 0:qn],
                                                                 start=(ki == 0), stop=(ki == nk - 1), **kw),
                 reads=[bconst, bpt[sb_]], writes=[bp[bank_l]], inc=True)

    QBLKS = [(0, 256, [0, 1])] + [(256 + 512 * j, 512, list(range(NT))) for j in range(4)]

    brope_t = bufs(2)

    def rope_block(bank, bank_r, Pm, Kp, Ct, St, t0, n, qraw, bqr, t1, t2, btab_, dst, bdst):
        c0 = t0 - CTX
        import os as _os
        mode = _os.environ.get("DBG_ROPE2", "full")
        if mode != "noP" or _os.environ.get("DBG_ACT", "1") == "1":
            B.op("act", lambda e: e.activation(out=qraw[0:Kp, 0:n], in_=ps[bank][0:Kp, 0:n], func=AF.Copy), reads=[bp[bank]], writes=[bqr])
        if mode != "noP":
            B.op("pe", lambda e: e.matmul(ps[bank_r][0:Kp, 0:n], lhsT=Pm[0:Kp, 0:Kp], rhs=qraw[0:Kp, 0:n], start=True, stop=True), reads=[bqr, bconst], writes=[bp[bank_r]])
        B.op("dve", lambda e: e.tensor_tensor(out=t1[0:Kp, 0:n], in0=ps[bank][0:Kp, 0:n], in1=Ct[0:Kp, c0:c0 + n], op=ALU.mult), reads=[bp[bank], btab_], writes=[brope_t[0]])
        if mode == "noP":
            B.op("dve", lambda e: e.tensor_copy(out=dst[0:Kp, t0:t0 + n], in_=t1[0:Kp, 0:n]), reads=[brope_t[0]], writes=[bdst])
            return
        B.op("dve", lambda e: e.tensor_tensor(out=t2[0:Kp, 0:n], in0=ps[bank_r][0:Kp, 0:n], in1=St[0:Kp, c0:c0 + n], op=ALU.mult), reads=[bp[bank_r], btab_], writes=[brope_t[1]])
        B.op(_os.environ.get("DBG_ROPE", "pool"), lambda e: e.tensor_tensor(out=dst[0:Kp, t0:t0 + n], in0=t1[0:Kp, 0:n], in1=t2[0:Kp, 0:n], op=ALU.add), reads=brope_t, writes=[bdst])

    def diffattn(b, l):
        a = Arena(MIX_BASE)
        w3 = a.bf(8 * 384).rearrange("p (c n) -> p c n", c=8)
        bw3 = Buf()
        qT = a.bf(T)
        kT = a.bf(T)
        vtok = a.bf(NT * 128).rearrange("p (i v) -> p i v", i=NT)
        Ct = a.f32(SEQ)
        St = a.f32(SEQ)
        qraw = a.bf(512)
        t1 = a.f32(512)
        t2 = a.f32(512)
        pt = [a.bf(512) for _ in range(3)]
        rc = a.f32(512)
        r = [a.f32(512) for _ in range(2)]
        dout = a.f32(T)
        sqb = a.f32(512)
        rst = a.f32(512)
        bout = a.bf(T)
        bq, bk, bv, btab, bqr, bt, brc, bdo, bsq, brs, bbo = [Buf() for _ in range(11)]
        bpt = bufs(3)
        br = bufs(2)
        B.dma("sp", Ct, dr["ropeCb"], writes=[btab])
        B.dma("sp", St, dr["ropeSb"], writes=[btab])
        lam_init = 0.8 - 0.6 * math.exp(-0.3 * (l + l0))
        import os as _os
        _sub = float(_os.environ.get("DBG_SUB", "9"))
        if _sub <= 0:
            return
        for hd in range(4):
            for g in range(3):
                load_w(w3[:, :, g * 128:(g + 1) * 128], dr["w_in"][l, :, 2560 + g * 512 + hd * 128:2560 + g * 512 + (hd + 1) * 128], bw3)
            for which, dstT, bdst in ((0, qT, bq), (1, kT, bk)):
                if _sub <= 0.2:
                    return
                for bi, (t0, n) in enumerate(BLKS):
                    if _sub <= 0.4 and bi >= 1:
                        return
                    if _sub <= 0.45 and bi >= 2:
                        return
                    bank = 3 + (bi % 2) * 2
                    proj_fm(bank, w3, (which * 128, which * 128 + 128), 8, hT, bh, t0, n, bw3)
                    if bi == 0:
                        B.op("act", lambda e, bank=bank, dstT=dstT, t0=t0, n=n: e.activation(out=dstT[:, t0:t0 + n], in_=ps[bank][:, 0:n], func=AF.Copy), reads=[bp[bank]], writes=[bdst])
                    else:
                        rope_block(bank, bank + 1, csb["Pb"], 128, Ct, St, t0, n, qraw, bqr, t1, t2, btab, dstT, bdst)
            if _sub <= 0.6:
                return
            for i in range(NT):
                bank = 3 + i % 2
                for c in range(8):
                    B.op("pe", lambda e, i=i, c=c, bank=bank: e.matmul(ps[bank][:, 0:128], lhsT=hT[:, c, i * 128:(i + 1) * 128], rhs=w3[:, c, 256:384], start=(c == 0), stop=(c == 7)),
                         reads=[bh[i], bw3], writes=[bp[bank]], inc=(c == 7))
                B.op("dve", lambda e, i=i, bank=bank: e.tensor_copy(out=vtok[:, i, :], in_=ps[bank][:, 0:128]), reads=[bp[bank]], writes=[bv])
            if _sub <= 1:
                return
            for qi, (qt0, qn, kts) in enumerate(QBLKS):
                if _sub <= 2 and qi > 0:
                    break
                for m in range(2):
                    bo_, bl_ = (3, 4) if m == 0 else (5, 6)
                    attention(qT, kT, 64, m * 64, vtok, (0, 128), 0.125, qt0, qn, kts, bo_, bl_, 0, 128, pt, bpt, bq, bk, bv)
                    B.op("dve", lambda e, bl_=bl_, qn=qn: e.reciprocal(out=rc[:, 0:qn], in_=ps[bl_][:, 0:qn]), reads=[bp[bl_]], writes=[brc])
                    B.op("dve", lambda e, bo_=bo_, qn=qn, m=m: e.tensor_tensor(out=r[m][:, 0:qn], in0=ps[bo_][:, 0:qn], in1=rc[:, 0:qn], op=ALU.mult), reads=[bp[bo_], brc], writes=[br[m]])
                B.op("dve", lambda e, qt0=qt0, qn=qn: e.scalar_tensor_tensor(out=dout[:, qt0:qt0 + qn], in0=r[1][:, 0:qn], scalar=small[:, 21:22], in1=r[0][:, 0:qn], op0=ALU.mult, op1=ALU.add),
                     reads=[br[0], br[1], bsmall], writes=[bdo])
                B.op("act", lambda e, qt0=qt0, qn=qn: e.activation(out=sqb[:, 0:qn], in_=dout[:, qt0:qt0 + qn], func=AF.Square), reads=[bdo], writes=[bsq])
                colnorm(7, [(sqb[:, 0:qn], 128)], qn, 1.0 / 128, rst[:, 0:qn], brs, [bsq])
                B.op("dve", lambda e, qt0=qt0, qn=qn, hd=hd: e.scalar_tensor_tensor(out=rst[:, 0:qn], in0=dout[:, qt0:qt0 + qn], scalar=small[:, 12 + hd:13 + hd], in1=rst[:, 0:qn], op0=ALU.mult, op1=ALU.mult),
                     reads=[bdo, brs, bsmall], writes=[brs])
                B.op("act", lambda e, qt0=qt0, qn=qn: e.activation(out=bout[:, qt0:qt0 + qn], in_=rst[:, 0:qn], func=AF.Copy, scale=1.0 - lam_init), reads=[brs], writes=[bbo])
            B.dma("sp", br_d[1, hd * 128:(hd + 1) * 128, :], bout, reads=[bbo])

    def mla(b, l):
        a = Arena(MIX_BASE)
        wc = a.bf(8 * 672).rearrange("p (c n) -> p c n", c=8)
        wuq = a.bf(3 * 768).rearrange("p (c n) -> p c n", c=3)
        wukv = a.bf(2 * 1024).rearrange("p (c n) -> p c n", c=2)
        bwc, bwq, bwkv = Buf(), Buf(), Buf()
        cqn = a.bf(3 * T).rearrange("p (c t) -> p c t", c=3)
        ckvn = a.bf(2 * T).rearrange("p (c t) -> p c t", c=2)
        bcq = bufs(NT)
        bckv = bufs(NT)
        R1 = a.off
        craw = a.f32(3 * 512).rearrange("p (c t) -> p c t", c=3)
        sq = a.f32(3 * 512).rearrange("p (c t) -> p c t", c=3)
        rstd = a.f32(512)
        bcraw, bsq, brstd = Buf(), Buf(), Buf()
        load_w(wc, dr["w_in"][l, :, 4096:4768], bwc)
        load_w(wuq, dr["mla_w_uq"][l], bwq)
        load_w(wukv, dr["mla_w_ukv"][l], bwkv)
        for name, ncol, c0, dst, bdst, gcol, dim in (("cq", 3, 0, cqn, bcq, 16, 384), ("ckv", 2, 384, ckvn, bckv, 19, 256)):
            for bi, (t0, n) in enumerate(BLKS):
                for cc in range(ncol):
                    bank = 3 + cc
                    proj_fm(bank, wc, (c0 + cc * 128, c0 + (cc + 1) * 128), 8, hT, bh, t0, n, bwc)
                    B.op("act", lambda e, cc=cc, n=n, bank=bank: e.activation(out=sq[:, cc, 0:n], in_=ps[bank][:, 0:n], func=AF.Square), reads=[bp[bank]], writes=[bsq])
                    B.op("dve", lambda e, cc=cc, n=n, bank=bank: e.tensor_copy(out=craw[:, cc, 0:n], in_=ps[bank][:, 0:n]), reads=[bp[bank]], writes=[bcraw])
                colnorm(7, [(sq[:, cc, 0:n], 128) for cc in range(ncol)], n, 1.0 / dim, rstd[:, 0:n], brstd, [bsq])
                for cc in range(ncol):
                    B.op("dve", lambda e, cc=cc, t0=t0, n=n, dst=dst, gcol=gcol: e.scalar_tensor_tensor(out=dst[:, cc, t0:t0 + n], in0=craw[:, cc, 0:n], scalar=small[:, gcol + cc:gcol + cc + 1], in1=rstd[:, 0:n], op0=ALU.mult, op1=ALU.mult),
                         reads=[bcraw, brstd, bsmall], writes=[bdst[i] for i in tiles_of(t0, n)])
        B.barrier()
        a = Arena(R1)
        qT = a.bf(T)
        kT = a.bf(T)
        vtok = a.bf(NT * 64).rearrange("p (i v) -> p i v", i=NT)
        Ct = a.f32(SEQ)
        St = a.f32(SEQ)
        qraw = a.bf(512)
        t1 = a.f32(512)
        t2 = a.f32(512)
        pt = [a.bf(512) for _ in range(3)]
        rc = a.f32(512)
        mout = a.bf(T)
        bq, bk, bv, btab, bqr, brc, bmo = [Buf() for _ in range(7)]
        bpt = bufs(3)
        B.dma("sp", Ct, dr["ropeCc"], writes=[btab])
        B.dma("sp", St, dr["ropeSc"], writes=[btab])
        scale = 96 ** -0.5
        for h in range(8):
            for bi, (t0, n) in enumerate(BLKS):
                bank = 3 + (bi % 2) * 2
                proj_fm(bank, wuq, (h * 96, h * 96 + 96), 3, cqn, bcq, t0, n, bwq, M=96)
                if bi == 0:
                    B.op("act", lambda e, bank=bank, t0=t0, n=n: e.activation(out=qT[0:96, t0:t0 + n], in_=ps[bank][0:96, 0:n], func=AF.Copy), reads=[bp[bank]], writes=[bq])
                else:
                    rope_block(bank, bank + 1, csb["Pc"], 96, Ct, St, t0, n, qraw, bqr, t1, t2, btab, qT, bq)
            for bi, (t0, n) in enumerate(BLKS):
                bank = 3 + (bi % 2) * 2
                proj_fm(bank, wukv, (h * 128, h * 128 + 64), 2, ckvn, bckv, t0, n, bwkv, M=64)
                proj_fm(bank, wc, (640, 672), 8, hT, bh, t0, n, bwc, M=32, out_part0=64, tile_pos=(0, 64))
                if bi == 0:
                    B.op("act", lambda e, bank=bank, t0=t0, n=n: e.activation(out=kT[0:96, t0:t0 + n], in_=ps[bank][0:96, 0:n], func=AF.Copy), reads=[bp[bank]], writes=[bk])
                else:
                    rope_block(bank, bank + 1, csb["Pc"], 96, Ct, St, t0, n, qraw, bqr, t1, t2, btab, kT, bk)
            for i in range(NT):
                bank = 3 + i % 2
                for c in range(2):
                    B.op("pe", lambda e, i=i, c=c, bank=bank, h=h: e.matmul(ps[bank][:, 0:64], lhsT=ckvn[:, c, i * 128:(i + 1) * 128], rhs=wukv[:, c, h * 128 + 64:h * 128 + 128], start=(c == 0), stop=(c == 1)),
                         reads=[bckv[i], bwkv], writes=[bp[bank]], inc=(c == 1))
                B.op("dve", lambda e, i=i, bank=bank: e.tensor_copy(out=vtok[:, i, :], in_=ps[bank][:, 0:64]), reads=[bp[bank]], writes=[bv])
            p0 = (h % 2) * 64
            for qi, (qt0, qn, kts) in enumerate(QBLKS):
                bo_, bl_ = (3, 4) if qi % 2 == 0 else (5, 6)
                attention(qT, kT, 96, 0, vtok, (0, 64), scale, qt0, qn, kts, bo_, bl_, p0, 64, pt, bpt, bq, bk, bv)
                B.op("dve", lambda e, bl_=bl_, qn=qn, p0=p0: e.reciprocal(out=rc[p0:p0 + 64, 0:qn], in_=ps[bl_][p0:p0 + 64, 0:qn]), reads=[bp[bl_]], writes=[brc])
                B.op("dve", lambda e, bo_=bo_, qn=qn, qt0=qt0, p0=p0: e.tensor_tensor(out=mout[p0:p0 + 64, qt0:qt0 + qn], in0=ps[bo_][p0:p0 + 64, 0:qn], in1=rc[p0:p0 + 64, 0:qn], op=ALU.mult),
                     reads=[bp[bo_], brc], writes=[bmo])
            if h % 2 == 1:
                B.dma("sp", br_d[2, (h // 2) * 128:(h // 2 + 1) * 128, :], mout, reads=[bmo])

    def merge(b, l):
        a = Arena(MIX_BASE)
        brs_ = a.bf(12 * T).rearrange("p (k t) -> p k t", k=12)
        bbr = Buf()
        wg = [a.bf(8 * 384).rearrange("p (c n) -> p c n", c=8) for _ in range(2)]
        wb = [a.bf(12 * 128).rearrange("p (k n) -> p k n", k=12) for _ in range(2)]
        bwg = bufs(2)
        bwb = bufs(2)
        gsb = [a.f32(512) for _ in range(3)]
        bgs = bufs(3)
        acc = a.f32(512)
        bacc = Buf()
        mrow = [a.bf(T)] * 2
        bmr = [Buf()] * 2
        for n_ in range(3):
            for cc in range(4):
                B.dma("sp", brs_[:, n_ * 4 + cc, :], br_d[n_, cc * 128:(cc + 1) * 128, :], writes=[bbr])
        for dc in range(8):
            j = dc % 2
            for n_ in range(3):
                load_w(wg[j][:, :, n_ * 128:(n_ + 1) * 128], dr["w_in"][l, :, 4768 + n_ * 1024 + dc * 128:4768 + n_ * 1024 + (dc + 1) * 128], bwg[j])
                B.dma("pool", wb[j][:, n_ * 4:(n_ + 1) * 4, :], dr["w_branch"][l, n_, :, dc * 128:(dc + 1) * 128].rearrange("(c p) n -> p c n", p=128), writes=[bwb[j]])
            for bi, (t0, n) in enumerate(BLKS):
                for n_ in range(3):
                    bank = n_
                    proj_fm(bank, wg[j], (n_ * 128, (n_ + 1) * 128), 8, hT, bh, t0, n, bwg[j])
                    B.op("act", lambda e, n_=n_, n=n, bank=bank: e.activation(out=gsb[n_][:, 0:n], in_=ps[bank][:, 0:n], func=AF.Sigmoid), reads=[bp[bank]], writes=[bgs[n_]])
                    bank2 = 3 + n_
                    for cc in range(4):
                        B.op("pe", lambda e, n_=n_, cc=cc, t0=t0, n=n, bank2=bank2, j=j: e.matmul(ps[bank2][:, 0:n], lhsT=wb[j][:, n_ * 4 + cc, :], rhs=brs_[:, n_ * 4 + cc, t0:t0 + n], start=(cc == 0), stop=(cc == 3)),
                             reads=[bwb[j], bbr], writes=[bp[bank2]], inc=(cc == 3))
                    if n_ == 0:
                        B.op("dve", lambda e, n=n, bank2=bank2: e.tensor_tensor(out=acc[:, 0:n], in0=ps[bank2][:, 0:n], in1=gsb[0][:, 0:n], op=ALU.mult), reads=[bp[bank2], bgs[0]], writes=[bacc])
                    else:
                        B.op("dve", lambda e, n=n, bank2=bank2, n_=n_: e.tensor_tensor(out=gsb[n_][:, 0:n], in0=ps[bank2][:, 0:n], in1=gsb[n_][:, 0:n], op=ALU.mult), reads=[bp[bank2], bgs[n_]], writes=[bgs[n_]])
                        if n_ == 1:
                            B.op("pool", lambda e, n=n: e.tensor_tensor(out=acc[:, 0:n], in0=acc[:, 0:n], in1=gsb[1][:, 0:n], op=ALU.add), reads=[bacc, bgs[1]], writes=[bacc])
                        else:
                            B.op("pool", lambda e, n=n, t0=t0, j=j: e.tensor_tensor(out=mrow[j][:, t0:t0 + n], in0=acc[:, 0:n], in1=gsb[2][:, 0:n], op=ALU.add), reads=[bacc, bgs[2]], writes=[bmr[j]])
            B.dma("sp", mrg_d[dc * 128:(dc + 1) * 128, :], mrow[j], reads=[bmr[j]])
        B.barrier()
        a = Arena(MIX_BASE)
        wo = a.bf(8 * D).rearrange("p (c n) -> p c n", c=8)
        bwo = Buf()
        gm = a.f32(2 * D).rearrange("p (r d) -> p r d", r=2)
        bgm = Buf()
        mt = [a.bf(8 * 128).rearrange("p (c t) -> p c t", c=8) for _ in range(2)]
        bmt = bufs(2)
        tmp = [a.f32(512) for _ in range(2)]
        btmp = bufs(2)
        load_w(wo, dr["w_out"][l], bwo)
        B.dma("sp", gm[:, 0, :], modv[l, b:b + 1, 2, :].to_broadcast([128, D]), writes=[bgm])
        B.dma("sp", gm[:, 1, :], modv[l, NB:NB + 1, 2, :].to_broadcast([128, D]), writes=[bgm])
        for i in range(NT):
            j = i % 2
            B.dma("sp", mt[j], mrg_d[:, i * 128:(i + 1) * 128].rearrange("(c p) t -> p c t", p=128), writes=[bmt[j]])
            r_ = 1 if i < 2 else 0
            for half in range(2):
                bank = (i % 2) * 2 + half
                for c in range(8):
                    B.op("pe", lambda e, c=c, j=j, half=half, bank=bank: e.matmul(ps[bank][:, :], lhsT=mt[j][:, c, :], rhs=wo[:, c, half * 512:(half + 1) * 512], start=(c == 0), stop=(c == 7)),
                         reads=[bmt[j], bwo], writes=[bp[bank]], inc=(c == 7))
                B.op("dve", lambda e, half=half, bank=bank, r_=r_: e.tensor_tensor(out=tmp[half], in0=ps[bank][:, :], in1=gm[:, r_, half * 512:(half + 1) * 512], op=ALU.mult),
                     reads=[bp[bank], bgm], writes=[btmp[half]])
                B.op("pool", lambda e, half=half, i=i: e.tensor_tensor(out=xs[:, i, half * 512:(half + 1) * 512], in0=xs[:, i, half * 512:(half + 1) * 512], in1=tmp[half], op=ALU.add),
                     reads=[btmp[half], bx[i]], writes=[bx[i]])

    def ffn(b, l, with_ctx):
        a = Arena(0)
        h2 = a.bf(NT * D).rearrange("p (i d) -> p i d", i=NT)
        bh2 = bufs(NT)
        slots = a.f32(T, parts=16)
        G = a.f32(T, parts=16)
        bsl, bG = Buf(), Buf()
        slT = a.f32(NT * 16).rearrange("p (i e) -> p i e", i=NT)
        bslT = Buf()
        R0 = a.off
        mod = a.f32(4 * D).rearrange("p (k d) -> p k d", k=4)
        bmod = Buf()
        rw = a.bf(8 * 16).rearrange("p (c e) -> p c e", c=8)
        brw = Buf()
        ssq = a.f32(NT)
        rstd = a.f32(NT)
        junk = a.bf(D)
        tmpf = [a.f32(D) for _ in range(2)]
        btf = bufs(2)
        h2T = [a.bf(8 * 128).rearrange("p (c t) -> p c t", c=8) for _ in range(2)]
        bh2T = bufs(2)
        aff = a.f32(T, parts=16)
        work = a.f32(T, parts=16)
        m8 = a.f32(8, parts=16)
        thr = a.f32(2, parts=16)
        baff, bwork, bm8, bthr, bss, brs, bj = [Buf() for _ in range(7)]
        for k, (r_, which) in enumerate(((b, 3), (b, 4), (NB, 3), (NB, 4))):
            B.dma("sp", mod[:, k, :], modv[l, r_:r_ + 1, which, :].to_broadcast([128, D]), writes=[bmod])
        B.dma("pool", rw, dr["router_w"][l].rearrange("(c p) e -> p c e", p=128), writes=[brw])
        for i in range(NT):
            B.op("act", lambda e, i=i: e.activation(out=junk, in_=xs[:, i, :], func=AF.Square, accum_out=ssq[:, i:i + 1]), reads=[bx[i]], writes=[bj, bss])
        B.op("act", lambda e: e.activation(out=rstd, in_=ssq, func=AF.Sqrt, scale=1.0 / D, bias=EPS), reads=[bss], writes=[brs])
        B.op("dve", lambda e: e.reciprocal(out=rstd, in_=rstd), reads=[brs], writes=[brs])
        for i in range(NT):
            j = i % 2
            k0 = 0 if i >= 2 else 2
            B.op("dve", lambda e, i=i, j=j, k0=k0: e.scalar_tensor_tensor(out=tmpf[j], in0=xs[:, i, :], scalar=rstd[:, i:i + 1], in1=mod[:, k0, :], op0=ALU.mult, op1=ALU.mult),
                 reads=[bx[i], brs, bmod], writes=[btf[j]])
            B.op("pool", lambda e, i=i, j=j, k0=k0: e.tensor_tensor(out=h2[:, i, :], in0=tmpf[j], in1=mod[:, k0 + 1, :], op=ALU.add), reads=[btf[j], bmod], writes=[bh2[i]])
            pbank = ps[j][:, 0:512].bitcast(BF16)
            for c in range(8):
                B.op("pe", lambda e, i=i, c=c, pbank=pbank: e.transpose(out=pbank[:, c * 128:(c + 1) * 128], in_=h2[:, i, c * 128:(c + 1) * 128], identity=ident_b[:]),
                     reads=[bh2[i], bconst], writes=[bp[j]], inc=(c == 7))
            B.op("act", lambda e, j=j, pbank=pbank: e.activation(out=h2T[j].rearrange("p c t -> p (c t)"), in_=pbank, func=AF.Copy), reads=[bp[j]], writes=[bh2T[j]])
            for c in range(8):
                B.op("pe", lambda e, j=j, c=c: e.matmul(ps[2 + j][0:16, 0:128], lhsT=rw[:, c, :], rhs=h2T[j][:, c, :], start=(c == 0), stop=(c == 7)),
                     reads=[brw, bh2T[j]], writes=[bp[2 + j]], inc=(c == 7))
            B.op("act", lambda e, i=i, j=j: e.activation(out=aff[:, i * 128:(i + 1) * 128], in_=ps[2 + j][0:16, 0:128], func=AF.Exp), reads=[bp[2 + j]], writes=[baff])
        for bi, (t0, n) in enumerate(BLKS):
            bank = 4 + bi % 2
            B.op("pe", lambda e, t0=t0, n=n, bank=bank: e.matmul(ps[bank][0:16, 0:n], lhsT=ones_f[0:16, 0:16], rhs=aff[:, t0:t0 + n], start=True, stop=True), reads=[baff, bconst], writes=[bp[bank]])
            B.op("dve", lambda e, t0=t0, n=n, bank=bank: e.reciprocal(out=work[:, t0:t0 + n], in_=ps[bank][0:16, 0:n]), reads=[bp[bank]], writes=[bwork])
        B.op("dve", lambda e: e.tensor_tensor(out=aff, in0=aff, in1=work, op=ALU.mult), reads=[baff, bwork], writes=[baff])
        B.op("dve", lambda e: e.tensor_copy(out=work, in_=aff), reads=[baff, bwork], writes=[bwork])
        for (c0, c1, nit, tcol) in ((CTX, T, CAP_L // 8, 0), (0, CTX, CAP_C // 8, 1)):
            for it in range(nit):
                B.op("dve", lambda e, c0=c0, c1=c1: e.max(out=m8, in_=work[:, c0:c1]), reads=[bwork], writes=[bm8])
                if it < nit - 1:
                    B.op("dve", lambda e, c0=c0, c1=c1: e.match_replace(out=work[:, c0:c1], in_to_replace=m8, in_values=work[:, c0:c1], imm_value=-1.0), reads=[bwork, bm8], writes=[bwork])
            B.op("dve", lambda e, tcol=tcol: e.tensor_copy(out=thr[:, tcol:tcol + 1], in_=m8[:, 7:8]), reads=[bm8], writes=[bthr])
        for (c0, c1, tcol, base) in ((CTX, T, 0, -1.0), (0, CTX, 1, float(CAP_L) - 1.0)):
            B.op("dve", lambda e, c0=c0, c1=c1, tcol=tcol: e.tensor_scalar(out=work[:, c0:c1], in0=aff[:, c0:c1], scalar1=thr[:, tcol:tcol + 1], scalar2=None, op0=ALU.is_ge), reads=[baff, bthr, bwork], writes=[bwork])
            B.op("dve", lambda e, c0=c0, c1=c1: e.tensor_tensor(out=G[:, c0:c1], in0=aff[:, c0:c1], in1=work[:, c0:c1], op=ALU.mult), reads=[baff, bwork], writes=[bG])
            B.op("dve", lambda e, c0=c0, c1=c1: e.tensor_scalar(out=aff[:, c0:c1], in0=aff[:, c0:c1], scalar1=0.0, scalar2=None, op0=ALU.is_ge), reads=[baff, bG], writes=[baff])
            B.op("dve", lambda e, c0=c0, c1=c1: e.tensor_tensor_scan(out=slots[:, c0:c1], data0=aff[:, c0:c1], data1=work[:, c0:c1], initial=0.0, op0=ALU.mult, op1=ALU.add), reads=[baff, bwork], writes=[bsl])
            B.op("dve", lambda e, c0=c0, c1=c1, base=base: e.scalar_tensor_tensor(out=slots[:, c0:c1], in0=slots[:, c0:c1], scalar=base + 1.0, in1=work[:, c0:c1], op0=ALU.add, op1=ALU.mult), reads=[bsl, bwork], writes=[bsl])
            B.op("dve", lambda e, c0=c0, c1=c1: e.tensor_scalar(out=slots[:, c0:c1], in0=slots[:, c0:c1], scalar1=-1.0, scalar2=None, op0=ALU.add), reads=[bsl], writes=[bsl])
        for i in range(NT):
            bank = 6 + i % 2
            B.op("pe", lambda e, i=i, bank=bank: e.transpose(out=ps[bank][:, 0:16], in_=slots[:, i * 128:(i + 1) * 128], identity=ident_f[0:16, 0:16]), reads=[bsl, bconst], writes=[bp[bank]])
            B.op("act", lambda e, i=i, bank=bank: e.activation(out=slT[:, i, :], in_=ps[bank][:, 0:16], func=AF.Copy), reads=[bp[bank]], writes=[bslT])
        if dbg:
            B.dma("sp", dbg_d["slots"], slots, reads=[bsl])
            B.dma("sp", dbg_d["G"], G, reads=[bG])
            for i in range(NT):
                B.dma("sp", dbg_d["h2"][i * 128:(i + 1) * 128, :], h2[:, i, :], reads=[bh2[i]])
        B.barrier()
        a = Arena(R0)
        gf = a.f32(2 * D).rearrange("p (r d) -> p r d", r=2)
        bgf = Buf()
        B.dma("sp", gf[:, 0, :], modv[l, b:b + 1, 5, :].to_broadcast([128, D]), writes=[bgf])
        B.dma("sp", gf[:, 1, :], modv[l, NB:NB + 1, 5, :].to_broadcast([128, D]), writes=[bgf])
        sel = a.bf(16 * 256 + 2 * 32)
        bsel = Buf()
        xsT = a.bf(8 * NSLOT).rearrange("p (c s) -> p c s", c=8)
        bxsT = Buf()
        act = a.bf(16 * NSLOT).rearrange("p (f s) -> p f s", f=16)
        bact = bufs(16)
        sa = [a.bf(NSLOT) for _ in range(2)]
        bsa = bufs(2)
        ye = a.bf(3 * D).rearrange("p (k d) -> p k d", k=3)
        bye = Buf()
        selT = a.bf(3 * T).rearrange("p (k t) -> p k t", k=3)
        bselT = Buf()
        Gb = a.f32(512)
        bGb = Buf()
        wgt = [a.bf(8 * 128).rearrange("p (c f) -> p c f", c=8) for _ in range(2)]
        wup = [a.bf(8 * 128).rearrange("p (c f) -> p c f", c=8) for _ in range(2)]
        wdn = [a.bf(D) for _ in range(2)]
        bwgt, bwup, bwdn = bufs(2), bufs(2), bufs(2)
        CCH = [(0, 128), (128, 128), (256, 32)]
        oneh = a.f32(128, parts=16)
        boh = Buf()
        wi = 0
        tile_lo = 0 if with_ctx else 2
        for e_ in range(NE):
            for i in range(NT):
                if i < 2:
                    dst = sel[:, 16 * 256 + i * 32:16 * 256 + (i + 1) * 32]
                    src = csb["iota_c"][:, 256:288]
                else:
                    dst = sel[:, (i - 2) * 256:(i - 1) * 256]
                    src = csb["iota_c"][:, 0:256]
                B.op("dve" if i % 2 == 0 else "pool", lambda e, dst=dst, src=src, i=i, e_=e_: e.tensor_scalar(out=dst, in0=src, scalar1=slT[:, i, e_:e_ + 1], scalar2=None, op0=ALU.is_equal),
                     reads=[bslT, bconst], writes=[bsel])
            B.op("dve", lambda e, e_=e_: e.tensor_scalar(out=oneh, in0=csb["pidx16"][:], scalar1=float(e_), scalar2=None, op0=ALU.is_equal), reads=[bconst], writes=[boh])
            for bi, (t0, n) in enumerate(BLKS):
                B.op("pe", lambda e, e_=e_, t0=t0, n=n: e.matmul(ps[0][:, 0:n], lhsT=oneh, rhs=slots[:, t0:t0 + n], start=True, stop=True), reads=[bsl, boh], writes=[bp[0]])
                B.op("pe", lambda e, e_=e_, t0=t0, n=n: e.matmul(ps[1][:, 0:n], lhsT=oneh, rhs=G[:, t0:t0 + n], start=True, stop=True), reads=[bG, boh], writes=[bp[1]])
                B.op("act", lambda e, n=n: e.activation(out=Gb[:, 0:n], in_=ps[1][:, 0:n], func=AF.Copy), reads=[bp[1]], writes=[bGb])
                for k in range(3):
                    B.op("dve", lambda e, k=k, t0=t0, n=n: e.scalar_tensor_tensor(out=selT[:, k, t0:t0 + n], in0=ps[0][:, 0:n], scalar=csb["iota_p"][:, k:k + 1], in1=Gb[:, 0:n], op0=ALU.is_equal, op1=ALU.mult),
                         reads=[bp[0], bGb, bconst], writes=[bselT])
            for c in range(8):
                bank = 6 + c % 2
                for i in range(2, NT):
                    B.op("pe", lambda e, c=c, i=i, bank=bank: e.matmul(ps[bank][:, 0:256], lhsT=h2[:, i, c * 128:(c + 1) * 128], rhs=sel[:, (i - 2) * 256:(i - 1) * 256], start=(i == 2), stop=(i == NT - 1)),
                         reads=[bh2[i], bsel], writes=[bp[bank]], inc=False)
                for i in range(2):
                    B.op("pe", lambda e, c=c, i=i, bank=bank: e.matmul(ps[bank][:, 256:288], lhsT=h2[:, i, c * 128:(c + 1) * 128], rhs=sel[:, 16 * 256 + i * 32:16 * 256 + (i + 1) * 32], start=(i == 0), stop=(i == 1)),
                         reads=[bh2[i], bsel], writes=[bp[bank]], inc=(i == 1))
                if c % 2 == 0:
                    B.op("act", lambda e, c=c, bank=bank: e.activation(out=xsT[:, c, :], in_=ps[bank][:, 0:NSLOT], func=AF.Copy), reads=[bp[bank]], writes=[bxsT])
                else:
                    B.op("dve", lambda e, c=c, bank=bank: e.tensor_copy(out=xsT[:, c, :], in_=ps[bank][:, 0:NSLOT]), reads=[bp[bank]], writes=[bxsT])
            def down(fc, jj):
                for k, (s0, sn) in enumerate(CCH):
                    for half in range(2):
                        bank = k * 2 + half
                        B.op("pe", lambda e, fc=fc, jj=jj, s0=s0, sn=sn, half=half, bank=bank: e.matmul(ps[bank][0:sn, :], lhsT=act[:, fc, s0:s0 + sn], rhs=wdn[jj][:, half * 512:(half + 1) * 512], start=(fc == 0), stop=(fc == 15)),
                             reads=[bact[fc], bwdn[jj]], writes=[bp[bank]], inc=(k == 2 and half == 1))
            prev = None
            for fc in range(16):
                jj = wi % 2
                wi += 1
                B.dma("pool", wgt[jj], dr["exp_w_gate"][l, e_, :, fc * 128:(fc + 1) * 128].rearrange("(c p) f -> p c f", p=128), writes=[bwgt[jj]])
                B.dma("pool", wup[jj], dr["exp_w_up"][l, e_, :, fc * 128:(fc + 1) * 128].rearrange("(c p) f -> p c f", p=128), writes=[bwup[jj]])
                B.dma("pool", wdn[jj], dr["exp_w_down"][l, e_, fc * 128:(fc + 1) * 128, :], writes=[bwdn[jj]])
                for c in range(8):
                    B.op("pe", lambda e, c=c, jj=jj: e.matmul(ps[6][:, 0:NSLOT], lhsT=wgt[jj][:, c, :], rhs=xsT[:, c, :], start=(c == 0), stop=(c == 7)), reads=[bwgt[jj], bxsT], writes=[bp[6]], inc=(c == 7))
                for c in range(8):
                    B.op("pe", lambda e, c=c, jj=jj: e.matmul(ps[7][:, 0:NSLOT], lhsT=wup[jj][:, c, :], rhs=xsT[:, c, :], start=(c == 0), stop=(c == 7)), reads=[bwup[jj], bxsT], writes=[bp[7]], inc=(c == 7))
                s_ = fc % 2
                B.op("act", lambda e, s_=s_: e.activation(out=sa[s_], in_=ps[6][:, 0:NSLOT], func=AF.Silu), reads=[bp[6]], writes=[bsa[s_]])
                B.op("dve", lambda e, s_=s_, fc=fc: e.tensor_tensor(out=act[:, fc, :], in0=ps[7][:, 0:NSLOT], in1=sa[s_], op=ALU.mult), reads=[bp[7], bsa[s_]], writes=[bact[fc]])
                if prev is not None:
                    down(*prev)
                prev = (fc, jj)
            down(*prev)
            for k, (s0, sn) in enumerate(CCH):
                r_ = 1 if k == 2 else 0
                for half in range(2):
                    bank = k * 2 + half
                    B.op("dve", lambda e, k=k, sn=sn, half=half, bank=bank, r_=r_: e.tensor_tensor(out=ye[0:sn, k, half * 512:(half + 1) * 512], in0=ps[bank][0:sn, :], in1=gf[0:sn, r_, half * 512:(half + 1) * 512], op=ALU.mult),
                         reads=[bp[bank], bgf], writes=[bye])
            for i in range(tile_lo, NT):
                ks = [2] if i < 2 else [0, 1]
                for half in range(2):
                    bank = (i % 3) * 2 + half
                    for kk_, k in enumerate(ks):
                        s0, sn = CCH[k]
                        B.op("pe", lambda e, i=i, k=k, sn=sn, half=half, bank=bank, kk_=kk_, ks=ks: e.matmul(ps[bank][:, :], lhsT=selT[0:sn, k, i * 128:(i + 1) * 128], rhs=ye[0:sn, k, half * 512:(half + 1) * 512], start=(kk_ == 0), stop=(kk_ == len(ks) - 1)),
                             reads=[bselT, bye], writes=[bp[bank]], inc=(kk_ == len(ks) - 1))
                    B.op("dve", lambda e, i=i, half=half, bank=bank: e.tensor_tensor(out=xs[:, i, half * 512:(half + 1) * 512], in0=ps[bank][:, :], in1=xs[:, i, half * 512:(half + 1) * 512], op=ALU.add),
                         reads=[bp[bank], bx[i]], writes=[bx[i]])

    def final(b):
        a = Arena(0)
        fg = a.f32(D)
        bfg = Buf()
        ssq = a.f32(NT)
        rstd = a.f32(NT)
        junk = a.bf(D)
        ot = [a.f32(D) for _ in range(2)]
        bot = bufs(2)
        bss, brs, bj = Buf(), Buf(), Buf()
        B.dma("sp", fg, dr["final_norm_g"].to_broadcast([128, D]), writes=[bfg])
        for i in range(2, NT):
            B.op("act", lambda e, i=i: e.activation(out=junk, in_=xs[:, i, :], func=AF.Square, accum_out=ssq[:, i:i + 1]), reads=[bx[i]], writes=[bj, bss])
        B.op("act", lambda e: e.activation(out=rstd[:, 2:NT], in_=ssq[:, 2:NT], func=AF.Sqrt, scale=1.0 / D, bias=EPS), reads=[bss], writes=[brs])
        B.op("dve", lambda e: e.reciprocal(out=rstd[:, 2:NT], in_=rstd[:, 2:NT]), reads=[brs], writes=[brs])
        for i in range(2, NT):
            j = i % 2
            B.op("dve", lambda e, i=i, j=j: e.scalar_tensor_tensor(out=ot[j], in0=xs[:, i, :], scalar=rstd[:, i:i + 1], in1=fg, op0=ALU.mult, op1=ALU.mult), reads=[bx[i], brs, bfg], writes=[bot[j]])
            B.dma("sp", out_d[b, (i - 2) * 128:(i - 1) * 128, :], ot[j], reads=[bot[j]])

    def dump_x(name):
        d = dbg_out(name, [T, D])
        for i in range(NT):
            B.dma("sp", d[i * 128:(i + 1) * 128, :], xs[:, i, :], reads=[bx[i]])

    def dump_hT(name):
        d = dbg_out(name, [D, T], BF16)
        for c in range(8):
            B.dma("sp", d[c * 128:(c + 1) * 128, :], hT[:, c, :], reads=bh)

    if dbg:
        dbg_out("slots", [16, T])
        dbg_out("G", [16, T])
        dbg_out("h2", [T, D], BF16)
    stages = ["norm1", "hgrn", "diff", "mla", "merge", "ffn"]
    last = stages.index(stop_after) if stop_after else len(stages) - 1
    for b in range(NB):
        load_x(b)
        for l in range(NL):
            layer_small(b, l)
            B.barrier()
            norm1(b, l)
            if dbg and l == dbg_layer and b == 0:
                dump_hT("hT")
            B.barrier()
            if last >= 1:
                hgrn(b, l)
                B.barrier()
            if last >= 2:
                diffattn(b, l)
                B.barrier()
            if last >= 3:
                mla(b, l)
                B.barrier()
            if last >= 4:
                merge(b, l)
                B.barrier()
                if dbg and l == dbg_layer and b == 0:
                    dump_x("x1")
            if last >= 5:
                ffn(b, l, with_ctx=(l + l0 < DEPTH - 1))
                B.barrier()
                if dbg and l == dbg_layer and b == 0:
                    dump_x("x2")
        if chain:
            for i in range(NT):
                dst = co_d[b, i * 128:(i + 1) * 128, :] if i < 2 else xo_d[b, (i - 2) * 128:(i - 1) * 128, :]
                B.dma("sp", dst, xs[:, i, :], reads=[bx[i]])
        if do_final:
            final(b)
        B.barrier()
    B.finish()
    return nc, B


def _fm(v, nchunk):
    v = np.asarray(v, np.float32)
    lead = v.shape[:-1]
    r = v.reshape(*lead, nchunk, 128)
    r = np.moveaxis(r, -1, 0)
    return np.ascontiguousarray(r.reshape(128, -1))


def _cT(cc):
    nr = cc.shape[0]
    nrp = nr + (nr % 2)
    ccp = np.zeros((nrp, D), np.float32)
    ccp[:nr] = cc
    return np.ascontiguousarray(ccp.T.reshape(8, 128, nrp).transpose(1, 0, 2).reshape(128, -1))


def make_inputs(inp, b0, NB, NLW=DEPTH):
    m = {}
    m["x"] = np.ascontiguousarray(inp["x"][b0:b0 + NB], dtype=np.float32)
    m["ctx"] = np.ascontiguousarray(inp["ctx"][b0:b0 + NB], dtype=np.float32)
    cc = np.concatenate([np.asarray(inp["c"][b0:b0 + NB], np.float32), np.asarray(inp["c_ctx"], np.float32)[None]], axis=0)
    m["cT"] = _cT(cc)
    for k in ("ada_w", "ada_b", "norm_mix_g", "norm_ffn_g", "w_in", "mla_w_uq", "mla_w_ukv", "w_branch", "w_out",
              "router_w", "exp_w_gate", "exp_w_up", "exp_w_down"):
        m[k] = np.ascontiguousarray(inp[k][:NLW], dtype=np.float32)
    m["diff_lambda"] = np.ascontiguousarray(np.asarray(inp["diff_lambda"], np.float32).reshape(DEPTH, 256)[:NLW])
    m["final_norm_g"] = np.ascontiguousarray(np.asarray(inp["final_norm_g"], np.float32).reshape(1, D))
    m["hlbT"] = _fm(np.asarray(inp["hgrn_lb"]).reshape(DEPTH, 2, 4, 128).reshape(DEPTH, 8, 128).reshape(DEPTH * 8, 128), 1)
    m["hngT"] = _fm(np.asarray(inp["hgrn_norm_g"]).reshape(DEPTH * 4, 128), 1)
    m["dngT"] = _fm(np.asarray(inp["diff_norm_g"]).reshape(DEPTH * 4, 128), 1)
    m["qngT"] = _fm(np.asarray(inp["mla_q_norm_g"]).reshape(DEPTH * 3, 128), 1)
    m["kvngT"] = _fm(np.asarray(inp["mla_kv_norm_g"]).reshape(DEPTH * 2, 128), 1)
    m.update(_consts())
    return m


_CACHE = {}


def kernel(**inputs):
    ncore = 8
    NB = inputs["x"].shape[0] // ncore
    key = (NB, DEPTH)
    if key not in _CACHE:
        _CACHE[key] = build(NB, DEPTH)
    nc, _ = _CACHE[key]
    base = make_inputs(inputs, 0, NB)
    in_maps = []
    for c in range(ncore):
        m = dict(base)
        m["x"] = np.ascontiguousarray(inputs["x"][c * NB:(c + 1) * NB], dtype=np.float32)
        m["ctx"] = np.ascontiguousarray(inputs["ctx"][c * NB:(c + 1) * NB], dtype=np.float32)
        cc = np.concatenate([np.asarray(inputs["c"][c * NB:(c + 1) * NB], np.float32), np.asarray(inputs["c_ctx"], np.float32)[None]], axis=0)
        m["cT"] = _cT(cc)
        in_maps.append(m)
    res = run_bass_kernel_spmd(nc, in_maps, core_ids=list(range(ncore)))
    return np.concatenate([np.asarray(r["out"], dtype=np.float32) for r in res.results], axis=0)
```
